# Optimizing a Trainium2 kernel written in Bass

```python
import math
import jax, jax.numpy as jnp
from jax import lax
import numpy as np

D_MODEL = 1024
BATCH = 8
SEQ = 8192
DEPTH = 1

D_MIX = D_MODEL
D_RNN = D_MIX // 2
D_ATTN = D_MIX - D_RNN
HEAD_DIM = 64
N_HEADS = D_ATTN // HEAD_DIM
N_RNN_BLOCKS = D_RNN // HEAD_DIM
RNN_BLOCK = D_RNN // N_RNN_BLOCKS
CONV_WIDTH = 4
LRU_C = 8.0
MOBA_BLOCK = 256
MOBA_TOPK = 3
Q_CHUNK = 128
ROPE_THETA = 500000.0
ROPE_DIM = HEAD_DIM // 4
D_FF = 2816
N_IN = 2 * D_RNN + 3 * D_ATTN
N_MOD = 9
ALPHA = (2.0 * DEPTH) ** 0.25
BETA = (8.0 * DEPTH) ** -0.25
LN_EPS = 1e-5
RMS_EPS = 1e-6
NEG_INF = -1e30

kernel_name = 'hymba_rglru_moba_macaron_deepnorm_adaln'


def layer_norm(x, g, b):
    xf = x.astype(jnp.float32)
    mu = jnp.mean(xf, axis=-1, keepdims=True)
    var = jnp.mean(jnp.square(xf - mu), axis=-1, keepdims=True)
    return ((xf - mu) * lax.rsqrt(var + LN_EPS) * g + b).astype(x.dtype)


def rms_norm(x, g):
    xf = x.astype(jnp.float32)
    return (xf * lax.rsqrt(jnp.mean(xf * xf, axis=-1, keepdims=True) + RMS_EPS) * g).astype(x.dtype)


def swiglu(h, w_gate, w_up, w_down):
    return (jax.nn.silu(h @ w_gate) * (h @ w_up)) @ w_down


def partial_rope(t, positions):
    half = ROPE_DIM // 2
    inv_freq = ROPE_THETA ** (-jnp.arange(half, dtype=jnp.float32) / half)
    ang = positions.astype(jnp.float32)[..., None] * inv_freq
    cos = jnp.cos(ang)[:, :, None, :]
    sin = jnp.sin(ang)[:, :, None, :]
    x1 = t[..., :half].astype(jnp.float32)
    x2 = t[..., half:ROPE_DIM].astype(jnp.float32)
    rot = jnp.concatenate([x1 * cos - x2 * sin, x2 * cos + x1 * sin], axis=-1).astype(t.dtype)
    return jnp.concatenate([rot, t[..., ROPE_DIM:]], axis=-1)


def causal_depthwise_conv(u, w, b):
    out = lax.conv_general_dilated(
        u, w[:, None, :], window_strides=(1,), padding=[(CONV_WIDTH - 1, 0)],
        dimension_numbers=('NWC', 'WIO', 'NWC'), feature_group_count=u.shape[-1])
    return out + b


def rg_lru(u, w_a, b_a, w_x, b_x, lam):
    B, S, _ = u.shape
    ub = u.reshape(B, S, N_RNN_BLOCKS, RNN_BLOCK)
    r = jax.nn.sigmoid(jnp.einsum('bsnd,nde->bsne', ub, w_a).reshape(B, S, D_RNN) + b_a)
    i = jax.nn.sigmoid(jnp.einsum('bsnd,nde->bsne', ub, w_x).reshape(B, S, D_RNN) + b_x)
    log_a = -LRU_C * r.astype(jnp.float32) * jax.nn.softplus(-lam.astype(jnp.float32))
    a = jnp.exp(log_a)
    inp = jnp.sqrt(-jnp.expm1(2.0 * log_a)) * (i * u).astype(jnp.float32)

    def combine(left, right):
        a1, b1 = left
        a2, b2 = right
        return a1 * a2, a2 * b1 + b2

    _, h = lax.associative_scan(combine, (a, inp), axis=1)
    return h.astype(u.dtype)


def moba_attention(q, k, v):
    B, S, H, Dh = q.shape
    nb = -(-S // MOBA_BLOCK)
    pad = nb * MOBA_BLOCK - S

    def to_blocks(t):
        t = jnp.pad(t, ((0, 0), (0, pad), (0, 0), (0, 0)))
        return t.reshape(B, nb, MOBA_BLOCK, H, Dh).transpose(0, 3, 1, 2, 4)

    k_blocks = to_blocks(k)
    v_blocks = to_blocks(v)
    k_mean = jnp.mean(k_blocks.astype(jnp.float32), axis=3)
    n_sel = min(MOBA_TOPK, nb)
    nc = S // Q_CHUNK
    scale = 1.0 / math.sqrt(Dh)
    q_chunks = q.reshape(B, nc, Q_CHUNK, H, Dh).transpose(0, 1, 3, 2, 4).reshape(B * nc, H, Q_CHUNK, Dh)
    blk_ids = jnp.arange(nb)
    key_in_blk = jnp.arange(MOBA_BLOCK)
    gather = jax.vmap(lambda t, idx: t[idx])

    def attend_chunk(args):
        q_c, step = args
        b = step // nc
        q_start = (step % nc) * Q_CHUNK
        qblk = q_start // MOBA_BLOCK
        q_in_blk = q_start % MOBA_BLOCK + jnp.arange(Q_CHUNK)
        kb = k_blocks[b]
        vb = v_blocks[b]
        km = k_mean[b]
        gate = jnp.einsum('hqd,hnd->hqn', q_c.astype(jnp.float32), km)
        gate = jnp.where(blk_ids[None, None, :] < qblk, gate, NEG_INF)
        _, idx = lax.top_k(gate, n_sel)
        valid = idx < qblk
        k_sel = gather(kb, idx)
        v_sel = gather(vb, idx)
        s_sel = jnp.einsum('hqd,hqnkd->hqnk', q_c, k_sel).astype(jnp.float32) * scale
        s_sel = jnp.where(valid[..., None], s_sel, NEG_INF).reshape(H, Q_CHUNK, n_sel * MOBA_BLOCK)
        k_own = lax.dynamic_index_in_dim(kb, qblk, axis=1, keepdims=False)
        v_own = lax.dynamic_index_in_dim(vb, qblk, axis=1, keepdims=False)
        s_own = jnp.einsum('hqd,hkd->hqk', q_c, k_own).astype(jnp.float32) * scale
        s_own = jnp.where(key_in_blk[None, None, :] <= q_in_blk[None, :, None], s_own, NEG_INF)
        p = jax.nn.softmax(jnp.concatenate([s_sel, s_own], axis=-1), axis=-1).astype(v.dtype)
        p_sel = p[..., :n_sel * MOBA_BLOCK].reshape(H, Q_CHUNK, n_sel, MOBA_BLOCK)
        p_own = p[..., n_sel * MOBA_BLOCK:]
        return (jnp.einsum('hqnk,hqnkd->hqd', p_sel, v_sel)
                + jnp.einsum('hqk,hkd->hqd', p_own, v_own))

    out = lax.map(attend_chunk, (q_chunks, jnp.arange(B * nc, dtype=jnp.int32)))
    return out.reshape(B, nc, H, Q_CHUNK, Dh).transpose(0, 1, 3, 2, 4).reshape(B, S, H * Dh)


def hybrid_mixer(h, positions, w_in, conv_w, conv_b, lru_wa, lru_ba, lru_wx, lru_bx, lru_lambda,
                 norm_rnn_g, norm_attn_g, w_out):
    B, S, _ = h.shape
    proj = h @ w_in
    u, g, q, k, v = jnp.split(
        proj, [D_RNN, 2 * D_RNN, 2 * D_RNN + D_ATTN, 2 * D_RNN + 2 * D_ATTN], axis=-1)
    u = causal_depthwise_conv(u, conv_w, conv_b)
    y_rnn = rg_lru(u, lru_wa, lru_ba, lru_wx, lru_bx, lru_lambda) * jax.nn.gelu(g)
    q = partial_rope(q.reshape(B, S, N_HEADS, HEAD_DIM), positions)
    k = partial_rope(k.reshape(B, S, N_HEADS, HEAD_DIM), positions)
    v = v.reshape(B, S, N_HEADS, HEAD_DIM)
    y_attn = moba_attention(q, k, v)
    y = jnp.concatenate([rms_norm(y_rnn, norm_rnn_g), rms_norm(y_attn, norm_attn_g)], axis=-1)
    return y @ w_out


def setup_inputs(seed: int = 0) -> dict:
    key = jax.random.key(seed)
    ks = jax.random.split(key, 32)
    f32 = jnp.float32
    L, D = DEPTH, D_MODEL

    def nrm(k, shape, fan_in, scale=1.0):
        return jax.random.normal(k, shape, f32) * (scale * fan_in ** -0.5)

    def gain(k, shape):
        return 1.0 + 0.02 * jax.random.normal(k, shape, f32)

    def small(k, shape):
        return 0.01 * jax.random.normal(k, shape, f32)

    a_c = jax.random.uniform(ks[20], (L, D_RNN), f32, 0.9, 0.999)
    s = a_c ** (1.0 / LRU_C)
    lru_lambda = jnp.log(s) - jnp.log1p(-s)
    return {
        'x': jax.random.normal(ks[0], (BATCH, SEQ, D), f32),
        'c': jax.random.normal(ks[1], (BATCH, D), f32),
        'positions': jnp.broadcast_to(jnp.arange(SEQ, dtype=jnp.int32), (BATCH, SEQ)),
        'ada_w': nrm(ks[2], (L, D, N_MOD * D), D, 0.3),
        'ada_b': small(ks[3], (L, N_MOD * D)),
        'ffn1_w_gate': nrm(ks[4], (L, D, D_FF), D),
        'ffn1_w_up': nrm(ks[5], (L, D, D_FF), D),
        'ffn1_w_down': nrm(ks[6], (L, D_FF, D), D_FF, BETA),
        'ln1_g': gain(ks[7], (L, D)),
        'ln1_b': small(ks[8], (L, D)),
        'w_in': nrm(ks[9], (L, D, N_IN), D),
        'conv_w': nrm(ks[10], (L, CONV_WIDTH, D_RNN), CONV_WIDTH),
        'conv_b': small(ks[11], (L, D_RNN)),
        'lru_wa': nrm(ks[12], (L, N_RNN_BLOCKS, RNN_BLOCK, RNN_BLOCK), RNN_BLOCK),
        'lru_ba': small(ks[13], (L, D_RNN)),
        'lru_wx': nrm(ks[14], (L, N_RNN_BLOCKS, RNN_BLOCK, RNN_BLOCK), RNN_BLOCK),
        'lru_bx': small(ks[15], (L, D_RNN)),
        'lru_lambda': lru_lambda,
        'norm_rnn_g': gain(ks[16], (L, D_RNN)),
        'norm_attn_g': gain(ks[17], (L, D_ATTN)),
        'w_out': nrm(ks[18], (L, D_MIX, D), D_MIX, BETA),
        'ln2_g': gain(ks[19], (L, D)),
        'ln2_b': small(ks[21], (L, D)),
        'ffn2_w_gate': nrm(ks[22], (L, D, D_FF), D),
        'ffn2_w_up': nrm(ks[23], (L, D, D_FF), D),
        'ffn2_w_down': nrm(ks[24], (L, D_FF, D), D_FF, BETA),
        'ln3_g': gain(ks[25], (L, D)),
        'ln3_b': small(ks[26], (L, D)),
    }


def reference(x, c, positions, ada_w, ada_b, ffn1_w_gate, ffn1_w_up, ffn1_w_down, ln1_g, ln1_b,
              w_in, conv_w, conv_b, lru_wa, lru_ba, lru_wx, lru_bx, lru_lambda, norm_rnn_g,
              norm_attn_g, w_out, ln2_g, ln2_b, ffn2_w_gate, ffn2_w_up, ffn2_w_down, ln3_g, ln3_b):
    c_act = jax.nn.silu(c)
    for l in range(DEPTH):
        mod = (c_act @ ada_w[l] + ada_b[l])[:, None, :]
        sh1, sc1, g1, sh2, sc2, g2, sh3, sc3, g3 = jnp.split(mod, N_MOD, axis=-1)
        h = x * (1.0 + sc1) + sh1
        x = layer_norm(ALPHA * x + 0.5 * (1.0 + g1) * swiglu(h, ffn1_w_gate[l], ffn1_w_up[l], ffn1_w_down[l]),
                       ln1_g[l], ln1_b[l])
        h = x * (1.0 + sc2) + sh2
        y = hybrid_mixer(h, positions, w_in[l], conv_w[l], conv_b[l], lru_wa[l], lru_ba[l], lru_wx[l],
                         lru_bx[l], lru_lambda[l], norm_rnn_g[l], norm_attn_g[l], w_out[l])
        x = layer_norm(ALPHA * x + (1.0 + g2) * y, ln2_g[l], ln2_b[l])
        h = x * (1.0 + sc3) + sh3
        x = layer_norm(ALPHA * x + 0.5 * (1.0 + g3) * swiglu(h, ffn2_w_gate[l], ffn2_w_up[l], ffn2_w_down[l]),
                       ln3_g[l], ln3_b[l])
    return x
```

```python
import numpy as np
from contextlib import ExitStack
import concourse.bass as bass
import concourse.mybir as mybir
from concourse.bass_utils import run_bass_kernel_spmd

F32 = mybir.dt.float32
BF16 = mybir.dt.bfloat16
I32 = mybir.dt.int32
AF = mybir.ActivationFunctionType
ALU = mybir.AluOpType
AX = mybir.AxisListType

D = 1024
DFF = 2816
NF = DFF // 128
NK = D // 128
N_IN = 2560
ALPHA = 2.0 ** 0.25
LN_EPS = 1e-5
RMS_EPS = 1e-6
SEQ = 8192
NCORES = 8
SKIP = set()

V_ADAB = 0
V_LN = 72
V_CONVW = 120
V_CONVB = 136
V_BA = 140
V_BX = 144
V_LAM = 148
V_GR = 152
V_GA = 156
NV = 160


class Hd:
    __slots__ = ("key", "val", "needed", "eng", "is_dma")


class Ph:
    __slots__ = ("real",)


class Prog:
    ENG = ("sync", "scalar", "vector", "gpsimd", "tensor")

    def __init__(self, nc):
        self.nc = nc
        self.streams = {e: [] for e in self.ENG}
        self.state = {}
        self.dmas = []
        self.last = {}
        self.keymap = {}
        self.dcnt = {}

    def capture(self):
        self._cap = []

    def end_capture(self):
        lst, self._cap = self._cap, None
        return lst

    def replay_interleaved(self, lists):
        n = max(len(l) for l in lists)
        for i in range(n):
            for l in lists:
                if i < len(l):
                    ph, eng, fn, reads, writes, dma_key, deps = l[i]
                    rd = [d.real if isinstance(d, Ph) else d for d in deps]
                    ph.real = self.op(eng, fn, reads=reads, writes=writes, dma_key=dma_key, deps=rd)

    def op(self, eng, fn, reads=(), writes=(), dma_key=None, deps=()):
        if getattr(self, "_cap", None) is not None:
            ph = Ph()
            self._cap.append((ph, eng, fn, list(reads), list(writes), dma_key, list(deps)))
            return ph
        h = Hd()
        h.eng = eng
        h.is_dma = dma_key is not None
        h.key = "E_" + eng
        h.needed = False
        h.val = None
        if h.is_dma:
            phys = self.keymap.setdefault(dma_key, len(self.keymap))
            h.key = f"D{phys}"
            self.dcnt[h.key] = self.dcnt.get(h.key, 0) + 16
            h.val = self.dcnt[h.key]
            h.needed = True
        dl = []
        for r in reads:
            st = self.state.get(r)
            if st is not None and st[0] is not None:
                dl.append(st[0])
        for w in writes:
            st = self.state.get(w)
            if st is not None:
                if st[0] is not None:
                    dl.append(st[0])
                dl.extend(st[1].values())
                dl.extend(st[2])
        dl.extend(d for d in deps if d is not None)
        seen = set()
        dd = []
        for d in dl:
            if id(d) not in seen:
                seen.add(id(d))
                d.needed = True
                dd.append(d)
        self.streams[eng].append((fn, dd, h))
        for r in reads:
            st = self.state.setdefault(r, [None, {}, []])
            if h.is_dma:
                st[2].append(h)
            else:
                st[1][eng] = h
        for w in writes:
            self.state[w] = [h, {}, []]
        if h.is_dma:
            self.dmas.append(h)
        elif fn is not None:
            self.last[eng] = h
        return h

    def barrier(self):
        deps = list(self.last.values()) + list(self.dmas)
        self.dmas = []
        for e in self.ENG:
            self.op(e, None, deps=deps)
        self.state = {}
        self.keymap = {}

    def emit(self, es):
        nc = self.nc
        cnt = {}
        for e in self.ENG:
            for (fn, dd, h) in self.streams[e]:
                if h.needed and not h.is_dma:
                    cnt[h.key] = cnt.get(h.key, 0) + 1
                    h.val = cnt[h.key]
        sems = {k: es.enter_context(nc.semaphore("s_" + k)) for k in list(cnt) + list(self.dcnt)}
        streams = self.streams

        def mk(ename):
            def body(eng):
                waited = {}
                for (fn, dd, h) in streams[ename]:
                    need = {}
                    for d in dd:
                        if waited.get(d.key, 0) < d.val:
                            need[d.key] = max(need.get(d.key, 0), d.val)
                    for k, v in need.items():
                        eng.wait_ge(sems[k], v)
                        waited[k] = v
                    if fn is None:
                        continue
                    ins = fn(eng)
                    if h.needed:
                        ins.then_inc(sems[h.key], 16 if h.is_dma else 1)
            return body

        with nc.Block() as blk:
            blk.sync(mk("sync"))
            blk.scalar(mk("scalar"))
            blk.vector(mk("vector"))
            blk.gpsimd(mk("gpsimd"))
            blk.tensor(mk("tensor"))


def build_nc(S=SEQ, debug=False, stop_after=None):
    nc = bass.Bass("TRN2", target_bir_lowering=False)
    P = Prog(nc)
    es = ExitStack()
    okind = "ExternalOutput" if debug else "Internal"

    def din(name, shape, dt=F32):
        return nc.dram_tensor(name, list(shape), dt, kind="ExternalInput").ap()

    xT = din("xT", [D, S])
    ccol = din("ccol", [128, NK])
    ada_w = din("ada_w", [D, 9 * D])
    vecs_d = din("vecs", [128, NV])
    w1g = din("w1g", [D, DFF])
    w1u = din("w1u", [D, DFF])
    w1d = din("w1d", [DFF, D])
    w2g = din("w2g", [D, DFF])
    w2u = din("w2u", [D, DFF])
    w2d = din("w2d", [DFF, D])
    w_in_d = din("w_in", [D, N_IN])
    w_out_d = din("w_out", [D, D])
    lru_wa_d = din("lru_wa", [8, 64, 64])
    lru_wx_d = din("lru_wx", [8, 64, 64])
    pos_d = din("pos", [1, S], I32)
    cst_d = din("cst", [128, 4])
    pswap_d = din("pswap", [128, 128], BF16)
    ident_d = din("ident", [128, 128], BF16)
    onehot_d = din("onehot", [32, S], BF16)
    negm_d = din("negm", [128, (S // 128) * 32])
    pastm_d = din("pastm", [128, (S // 128) * 32])
    cm_d = din("cm", [128, 4 * 512], BF16)
    outT = nc.dram_tensor("outT", [D, S], F32, kind="ExternalOutput").ap()
    x2T = nc.dram_tensor("x2T", [D, S], F32, kind=okind).ap()
    YT = nc.dram_tensor("YT", [D, S], F32, kind=okind).ap()
    qT = nc.dram_tensor("qT", [512, S], BF16, kind=okind).ap()
    kT = nc.dram_tensor("kT", [512, S], BF16, kind=okind).ap()
    Vs = nc.dram_tensor("Vs", [8, 128, S // 128, 128], BF16, kind=okind).ap()
    kmD = nc.dram_tensor("kmD", [512, 32], BF16, kind=okind).ap()
    x1T = nc.dram_tensor("x1T", [D, S], F32, kind=okind).ap()
    modT_d = nc.dram_tensor("modT_d", [128, 72], F32, kind=okind).ap()

    T = 256
    NT = S // T

    vecs = es.enter_context(nc.sbuf_tensor("vecs_sb", [128, NV], F32))
    modT = es.enter_context(nc.sbuf_tensor("modT", [128, 72], F32))
    opsc = es.enter_context(nc.sbuf_tensor("opsc", [128, 24], F32))
    gsc = es.enter_context(nc.sbuf_tensor("gsc", [128, 24], F32))
    ones_ln = es.enter_context(nc.sbuf_tensor("ones_ln", [128, 128], BF16))
    nhalf = es.enter_context(nc.sbuf_tensor("nhalf", [128, 512], F32))
    negpi = es.enter_context(nc.sbuf_tensor("negpi", [128, 1], F32))

    P.op("sync", lambda e: e.dma_start(out=vecs[:, :], in_=vecs_d), writes=["vecs"], dma_key="d_vecs")
    P.op("gpsimd", lambda e: e.memset(ones_ln[:, :], 1.0 / D), writes=["ones_ln"])
    P.op("gpsimd", lambda e: e.memset(nhalf[:, :], -0.5), writes=["nhalf"])
    P.op("gpsimd", lambda e: e.memset(negpi[:, :], -3.1415925), writes=["negpi"])

    with ExitStack() as ps0:
        ps = [ps0.enter_context(nc.psum_tensor("p0ps0", [128, 512], F32))]
        cc = ps0.enter_context(nc.sbuf_tensor("cc", [128, NK], F32))
        cact = ps0.enter_context(nc.sbuf_tensor("cact", [128, NK], F32))
        aw = [ps0.enter_context(nc.sbuf_tensor(f"aw{i}", [128, NK, D], F32)) for i in range(2)]
        P.op("sync", lambda e: e.dma_start(out=cc[:, :], in_=ccol), writes=["cc"], dma_key="d_cc")
        P.op("scalar", lambda e: e.activation(out=cact[:, :], in_=cc[:, :], func=AF.Silu),
             reads=["cc"], writes=["cact"])
        aw_d = ada_w.rearrange("(kk p) n -> p kk n", p=128)
        for i in range(9):
            sl = i % 2
            for kk in range(NK):
                P.op("sync", lambda e, sl=sl, i=i, kk=kk: e.dma_start(
                    out=aw[sl][:, kk, :], in_=aw_d[:, kk, i * D:(i + 1) * D]),
                    writes=[f"aw{sl}_{kk}"], dma_key=f"d_aw{sl}_{kk}")

            def mm(e, sl=sl, i=i):
                ins = None
                for c in range(NK):
                    for kk in range(NK):
                        ins = e.matmul(ps[0][:, i * 8 + c:i * 8 + c + 1],
                                       lhsT=aw[sl][:, kk, c * 128:(c + 1) * 128],
                                       rhs=cact[:, kk:kk + 1], start=(kk == 0), stop=(kk == NK - 1))
                return ins
            P.op("tensor", mm, reads=["cact"] + [f"aw{sl}_{kk}" for kk in range(NK)], writes=["ps0"])
        P.op("vector", lambda e: e.tensor_tensor(out=modT[:, :], in0=ps[0][:, 0:72],
                                                 in1=vecs[:, V_ADAB:V_ADAB + 72], op=ALU.add),
             reads=["ps0", "vecs"], writes=["modT"])
        for i in range(3):
            P.op("vector", lambda e, i=i: e.tensor_scalar(
                out=opsc[:, i * 8:(i + 1) * 8], in0=modT[:, (3 * i + 1) * 8:(3 * i + 2) * 8],
                scalar1=1.0, scalar2=None, op0=ALU.add), reads=["modT"], writes=[f"opsc{i}"])
            gmul = (1.0 if i == 1 else 0.5) / ALPHA
            P.op("vector", lambda e, i=i, gmul=gmul: e.tensor_scalar(
                out=gsc[:, i * 8:(i + 1) * 8], in0=modT[:, (3 * i + 2) * 8:(3 * i + 3) * 8],
                scalar1=1.0, scalar2=gmul, op0=ALU.add, op1=ALU.mult), reads=["modT"], writes=[f"gsc{i}"])
        if debug:
            P.op("sync", lambda e: e.dma_start(out=modT_d, in_=modT[:, :]), reads=["modT"], dma_key="d_dbg")
        P.barrier()

    def ffn_phase(tag, src, dst, wg_d, wu_d, wd_d, mi, ln_col):
        sh_c = lambda k: modT[:, (3 * mi) * 8 + k:(3 * mi) * 8 + k + 1]
        sc_c = lambda k: opsc[:, mi * 8 + k:mi * 8 + k + 1]
        g_c = lambda k: gsc[:, mi * 8 + k:mi * 8 + k + 1]
        lg_c = lambda k: vecs[:, ln_col + k:ln_col + k + 1]
        lb_c = lambda k: vecs[:, ln_col + 8 + k:ln_col + 8 + k + 1]
        eps = LN_EPS / (ALPHA * ALPHA)
        with ExitStack() as pes:
            ps = [pes.enter_context(nc.psum_tensor(tag + f"ps{i}", [128, 512], F32)) for i in range(8)]
            wg = pes.enter_context(nc.sbuf_tensor(tag + "wg", [128, NK, DFF], BF16))
            wu = pes.enter_context(nc.sbuf_tensor(tag + "wu", [128, NK, DFF], BF16))
            wd = pes.enter_context(nc.sbuf_tensor(tag + "wd", [128, NF, D], BF16))
            xb = [pes.enter_context(nc.sbuf_tensor(tag + f"xb{i}", [128, NK, T], F32)) for i in range(3)]
            hb = pes.enter_context(nc.sbuf_tensor(tag + "hb", [128, NK, T], BF16))
            act = pes.enter_context(nc.sbuf_tensor(tag + "act", [128, NF, T], BF16))
            sgt = [pes.enter_context(nc.sbuf_tensor(tag + f"sg{i}", [128, T], BF16)) for i in range(2)]
            zb = [pes.enter_context(nc.sbuf_tensor(tag + f"zb{i}", [128, T], BF16)) for i in range(2)]
            zq = [pes.enter_context(nc.sbuf_tensor(tag + f"zq{i}", [128, T], BF16)) for i in range(2)]
            mean = pes.enter_context(nc.sbuf_tensor(tag + "mean", [128, T], F32))
            var = pes.enter_context(nc.sbuf_tensor(tag + "var", [128, T], F32))
            rstd = pes.enter_context(nc.sbuf_tensor(tag + "rstd", [128, T], F32))

            for kk in range(NK):
                P.op("gpsimd", lambda e, kk=kk: e.dma_start(out=wg[:, kk, :], in_=wg_d[kk * 128:(kk + 1) * 128, :]),
                     writes=[f"wg{kk}"], dma_key=f"d_wg{kk}")
                P.op("gpsimd", lambda e, kk=kk: e.dma_start(out=wu[:, kk, :], in_=wu_d[kk * 128:(kk + 1) * 128, :]),
                     writes=[f"wu{kk}"], dma_key=f"d_wu{kk}")
            for j in range(NF):
                P.op("gpsimd", lambda e, j=j: e.dma_start(out=wd[:, j, :], in_=wd_d[j * 128:(j + 1) * 128, :]),
                     writes=[f"wd{j}"], dma_key=f"d_wd{j}")
            src_v = src.rearrange("(k p) s -> p k s", p=128)
            dst_v = dst.rearrange("(k p) s -> p k s", p=128)

            def load(m):
                sl = m % 3
                P.op("sync", lambda e: e.dma_start(out=xb[sl][:, :, :], in_=src_v[:, :, m * T:(m + 1) * T]),
                     writes=[f"x{sl}_{k}" for k in range(NK)], dma_key=f"d_x{sl}")

            def make_h(m):
                sl = m % 3
                for k in range(NK):
                    P.op("gpsimd", lambda e, k=k: e.tensor_scalar(
                        out=hb[:, k, :], in0=xb[sl][:, k, :], scalar1=sc_c(k), scalar2=sh_c(k),
                        op0=ALU.mult, op1=ALU.add),
                        reads=[f"x{sl}_{k}", "modT", "opsc0", "opsc2"], writes=[f"h{k}"])

            load(0)
            if NT > 1:
                load(1)
            make_h(0)
            pending = [None]

            def do_tile(m):
                sl = m % 3
                P.capture()
                for j in range(NF):
                    pg = ps[j % 2]
                    pu = ps[2 + j % 2]

                    def mmg(e, j=j, pg=pg):
                        ins = None
                        for k in range(NK):
                            ins = e.matmul(pg[:, 0:T], lhsT=wg[:, k, j * 128:(j + 1) * 128], rhs=hb[:, k, :],
                                           start=(k == 0), stop=(k == NK - 1))
                        return ins

                    def mmu(e, j=j, pu=pu):
                        ins = None
                        for k in range(NK):
                            ins = e.matmul(pu[:, 0:T], lhsT=wu[:, k, j * 128:(j + 1) * 128], rhs=hb[:, k, :],
                                           start=(k == 0), stop=(k == NK - 1))
                        return ins
                    hr = [f"h{k}" for k in range(NK)]
                    P.op("tensor", mmg, reads=hr + [f"wg{k}" for k in range(NK)], writes=[f"pg{j % 2}"])
                    P.op("tensor", mmu, reads=hr + [f"wu{k}" for k in range(NK)], writes=[f"pu{j % 2}"])
                    P.op("scalar", lambda e, j=j, pg=pg: e.activation(out=sgt[j % 2][:, :], in_=pg[:, 0:T], func=AF.Silu),
                         reads=[f"pg{j % 2}"], writes=[f"sg{j % 2}"])
                    P.op("vector", lambda e, j=j, pu=pu: e.tensor_tensor(
                        out=act[:, j, :], in0=sgt[j % 2][:, :], in1=pu[:, 0:T], op=ALU.mult),
                        reads=[f"sg{j % 2}", f"pu{j % 2}"], writes=[f"act{j}"])
                gu_ops = P.end_capture()
                P.replay_interleaved([gu_ops, pending[0]] if pending[0] else [gu_ops])
                pending[0] = None
                if m + 2 < NT:
                    load(m + 2)
                if m + 1 < NT:
                    make_h(m + 1)
                def stats(dk):
                    P.op("tensor", lambda e, dk=dk: e.matmul(ps[6][:, 0:T], lhsT=ones_ln[:, :], rhs=zb[dk % 2][:, :],
                                                             start=(dk == 0), stop=(dk == NK - 1)),
                         reads=[f"zb{dk % 2}", "ones_ln"], writes=["pmean"])
                    P.op("tensor", lambda e, dk=dk: e.matmul(ps[7][:, 0:T], lhsT=ones_ln[:, :], rhs=zq[dk % 2][:, :],
                                                             start=(dk == 0), stop=(dk == NK - 1)),
                         reads=[f"zq{dk % 2}", "ones_ln"], writes=["pez2"])
                for dk in range(NK):
                    py = ps[4 + dk % 2]

                    def mmd(e, dk=dk, py=py):
                        ins = None
                        for j in range(NF):
                            ins = e.matmul(py[:, 0:T], lhsT=wd[:, j, dk * 128:(dk + 1) * 128], rhs=act[:, j, :],
                                           start=(j == 0), stop=(j == NF - 1))
                        return ins
                    P.op("tensor", mmd, reads=[f"act{j}" for j in range(NF)] + [f"wd{j}" for j in range(NF)],
                         writes=[f"py{dk % 2}"])
                    P.op("vector", lambda e, dk=dk, py=py: e.scalar_tensor_tensor(
                        out=xb[sl][:, dk, :], in0=py[:, 0:T], scalar=g_c(dk), in1=xb[sl][:, dk, :],
                        op0=ALU.mult, op1=ALU.add),
                        reads=[f"py{dk % 2}", f"x{sl}_{dk}", "gsc0", "gsc2"], writes=[f"x{sl}_{dk}"])
                    P.op("scalar", lambda e, dk=dk: e.activation(out=zb[dk % 2][:, :], in_=xb[sl][:, dk, :], func=AF.Copy),
                         reads=[f"x{sl}_{dk}"], writes=[f"zb{dk % 2}"])
                    P.op("gpsimd", lambda e, dk=dk: e.tensor_tensor(
                        out=zq[dk % 2][:, :], in0=xb[sl][:, dk, :], in1=xb[sl][:, dk, :], op=ALU.mult),
                        reads=[f"x{sl}_{dk}"], writes=[f"zq{dk % 2}"])
                    if dk >= 1:
                        stats(dk - 1)
                stats(NK - 1)
                P.capture()
                P.op("scalar", lambda e: e.activation(out=mean[:, :], in_=ps[6][:, 0:T], func=AF.Copy),
                     reads=["pmean"], writes=["mean"])
                P.op("vector", lambda e: e.tensor_tensor(out=var[:, :], in0=mean[:, :], in1=mean[:, :], op=ALU.mult),
                     reads=["mean"], writes=["var"])
                P.op("vector", lambda e: e.scalar_tensor_tensor(
                    out=var[:, :], in0=ps[7][:, 0:T], scalar=eps, in1=var[:, :], op0=ALU.add, op1=ALU.subtract),
                    reads=["pez2", "var"], writes=["var"])
                P.op("scalar", lambda e: e.activation(out=rstd[:, :], in_=var[:, :], func=AF.Ln), reads=["var"], writes=["rstd"])
                P.op("scalar", lambda e: e.activation(out=rstd[:, :], in_=rstd[:, :], func=AF.Exp, scale=-0.5), reads=["rstd"], writes=["rstd"])
                for dk in range(NK):
                    P.op("vector", lambda e, dk=dk: e.tensor_tensor(
                        out=xb[sl][:, dk, :], in0=xb[sl][:, dk, :], in1=mean[:, :], op=ALU.subtract),
                        reads=[f"x{sl}_{dk}", "mean"], writes=[f"x{sl}_{dk}"])
                for dk in range(NK):
                    P.op("vector", lambda e, dk=dk: e.tensor_tensor(
                        out=xb[sl][:, dk, :], in0=xb[sl][:, dk, :], in1=rstd[:, :], op=ALU.mult),
                        reads=[f"x{sl}_{dk}", "rstd"], writes=[f"x{sl}_{dk}"])
                for dk in range(NK):
                    P.op("gpsimd", lambda e, dk=dk: e.tensor_scalar(
                        out=xb[sl][:, dk, :], in0=xb[sl][:, dk, :], scalar1=lg_c(dk), scalar2=lb_c(dk),
                        op0=ALU.mult, op1=ALU.add),
                        reads=[f"x{sl}_{dk}", "vecs"], writes=[f"x{sl}_{dk}"])
                P.op("sync", lambda e, m=m: e.dma_start(out=dst_v[:, :, m * T:(m + 1) * T], in_=xb[sl][:, :, :]),
                     reads=[f"x{sl}_{k}" for k in range(NK)], dma_key=f"d_o{sl}")
                pending[0] = P.end_capture()
            for m in range(NT):
                do_tile(m)
            P.replay_interleaved([pending[0]])
            P.barrier()

    TB = 512
    NTB = S // TB
    NCH = S // 128
    NBLK = S // 256

    def mixer_proj_phase():
        with ExitStack() as pes:
            sbt = lambda name, shape, dt=F32: pes.enter_context(nc.sbuf_tensor("b1" + name, shape, dt))
            ps = [pes.enter_context(nc.psum_tensor(f"b1ps{i}", [128, 512], F32)) for i in range(8)]
            win = sbt("win", [128, NK, N_IN], BF16)
            wab = sbt("wab", [128, 4, 128], BF16)
            wxb = sbt("wxb", [128, 4, 128], BF16)
            psw = sbt("psw", [128, 128], BF16)
            cst = sbt("cst", [128, 4], F32)
            clt = sbt("clt", [128, 12], F32)
            half = sbt("half", [128, TB], F32)
            xb = [sbt(f"xb{i}", [128, NK, TB], F32) for i in range(2)]
            hb = sbt("hb", [128, NK, TB], BF16)
            posi = sbt("posi", [128, TB], I32)
            rt = {n: sbt("rt" + n, [128, TB], F32) for n in ("ts", "tc", "f1", "f2", "m1", "m2", "sin", "cos")}
            rti = [sbt(f"rti{i}", [128, TB], I32) for i in range(2)]
            ub = sbt("ub", [128, 4, TB + 3], F32)
            uc = sbt("uc", [128, 4, TB], F32)
            ucb = sbt("ucb", [128, 4, TB], BF16)
            tmp = {n: [sbt(f"{n}{i}", [128, TB], F32) for i in range(2)]
                   for n in ("rr", "ii", "aa", "a2", "mu", "inp", "hs", "gx", "yr", "t1", "t2")}
            qb = [sbt(f"qb{i}", [128, TB], BF16) for i in range(2)]
            qr = sbt("qr", [128, 4, TB], BF16)
            kr = sbt("kr", [128, 4, TB], BF16)
            vaug = sbt("vaug", [128, 8, 4, 128], BF16)
            carry = sbt("carry", [128, 4], F32)
            kmT = sbt("kmT", [128, 4, NBLK], F32)
            kmb = sbt("kmb", [128, 4, 32], BF16)

            for kk in range(NK):
                P.op("gpsimd", lambda e, kk=kk: e.dma_start(out=win[:, kk, :], in_=w_in_d[kk * 128:(kk + 1) * 128, :]),
                     writes=[f"win{kk}"], dma_key=f"d_win{kk}")
            P.op("vector", lambda e: e.memset(wab[:, :, :], 0.0), writes=["wab"])
            P.op("vector", lambda e: e.memset(wxb[:, :, :], 0.0), writes=["wxb"])
            for n in range(8):
                c, hh = n // 2, n % 2
                P.op("gpsimd", lambda e, n=n, c=c, hh=hh: e.dma_start(
                    out=wab[hh * 64:(hh + 1) * 64, c, hh * 64:(hh + 1) * 64], in_=lru_wa_d[n, :, :]),
                    reads=[], writes=["wab"], dma_key=f"d_wa{n}")
                P.op("gpsimd", lambda e, n=n, c=c, hh=hh: e.dma_start(
                    out=wxb[hh * 64:(hh + 1) * 64, c, hh * 64:(hh + 1) * 64], in_=lru_wx_d[n, :, :]),
                    reads=[], writes=["wxb"], dma_key=f"d_wx{n}")
            P.op("sync", lambda e: e.dma_start(out=psw[:, :], in_=pswap_d), writes=["psw"], dma_key="d_psw")
            P.op("sync", lambda e: e.dma_start(out=cst[:, :], in_=cst_d), writes=["cst"], dma_key="d_cst")
            P.op("vector", lambda e: e.memset(half[:, :], 0.5), writes=["half"])
            P.op("vector", lambda e: e.memset(ub[:, :, :], 0.0), writes=[f"ub{c}" for c in range(4)])
            P.op("vector", lambda e: e.memset(carry[:, :], 0.0), writes=[f"carry{c}" for c in range(4)])
            P.op("vector", lambda e: e.memset(vaug[:, :, :, :], 1.0), writes=["vaug"])
            P.op("vector", lambda e: e.memset(kmb[:, :, :], 0.0), writes=["kmb"])
            P.op("scalar", lambda e: e.activation(out=clt[:, 0:4], in_=vecs[:, V_LAM:V_LAM + 4], func=AF.Exp, scale=-1.0),
                 reads=["vecs"], writes=["clt0"])
            P.op("scalar", lambda e: e.activation(out=clt[:, 0:4], in_=clt[:, 0:4], func=AF.Ln, bias=1.0, scale=1.0),
                 reads=["clt0"], writes=["clt0"])
            P.op("vector", lambda e: e.tensor_scalar(out=clt[:, 4:8], in0=clt[:, 0:4], scalar1=-8.0, scalar2=None, op0=ALU.mult),
                 reads=["clt0"], writes=["clt1"])
            P.op("vector", lambda e: e.tensor_scalar(out=clt[:, 8:12], in0=clt[:, 0:4], scalar1=-16.0, scalar2=None, op0=ALU.mult),
                 reads=["clt0"], writes=["clt2"])

            src_v = x1T.rearrange("(k p) s -> p k s", p=128)
            yT_v = YT.rearrange("(k p) s -> p k s", p=128)
            qT_v = qT.rearrange("(k p) s -> p k s", p=128)
            kT_v = kT.rearrange("(k p) s -> p k s", p=128)
            Vs_v = Vs.rearrange("h p j e -> p h j e")
            vcol = lambda col, c: vecs[:, col + c:col + c + 1]
            bankc = [0]

            def nbank():
                b = bankc[0] % 4
                bankc[0] += 1
                return b

            def load(m):
                sl = m % 2
                P.op("sync", lambda e: e.dma_start(out=xb[sl][:, :, :], in_=src_v[:, :, m * TB:(m + 1) * TB]),
                     writes=[f"x{sl}_{k}" for k in range(NK)], dma_key=f"d_x{sl}")

            def proj(m, col0, key):
                b = nbank()

                def mm(e, b=b):
                    ins = None
                    for k in range(NK):
                        ins = e.matmul(ps[b][:, 0:TB], lhsT=win[:, k, col0:col0 + 128], rhs=hb[:, k, :],
                                       start=(k == 0), stop=(k == NK - 1))
                    return ins
                P.op("tensor", mm, reads=[f"h{k}" for k in range(NK)] + [f"win{k}" for k in range(NK)], writes=[f"pp{b}"])
                return b

            def do_tile(m):
                sl = m % 2
                if m + 1 < NTB:
                    load(m + 1)
                tsl = slice(m * TB, (m + 1) * TB)
                for k in range(NK):
                    P.op("gpsimd", lambda e, k=k: e.tensor_scalar(
                        out=hb[:, k, :], in0=xb[sl][:, k, :], scalar1=opsc[:, 8 + k:9 + k], scalar2=modT[:, 24 + k:25 + k],
                        op0=ALU.mult, op1=ALU.add), reads=[f"x{sl}_{k}"], writes=[f"h{k}"])
                def cap(f, *a):
                    P.capture()
                    f(*a)
                    return P.end_capture()
                P.replay_interleaved([cap(rope_tables, m, tsl), cap(rnn_part, m, tsl, sl, 0), cap(rnn_part, m, tsl, sl, 1)])
                P.replay_interleaved([cap(rnn_part, m, tsl, sl, 2), cap(rnn_part, m, tsl, sl, 3)])
                for which in ("q", "k"):
                    P.replay_interleaved([cap(qk_part, m, tsl, sl, which, 0), cap(qk_part, m, tsl, sl, which, 1)])
                    P.replay_interleaved([cap(qk_part, m, tsl, sl, which, 2), cap(qk_part, m, tsl, sl, which, 3)])
                    qk_store(m, tsl, which)
                v_part(m, tsl, sl)

            def rope_tables(m, tsl):
                P.op("sync", lambda e: e.dma_start(out=posi[:, :], in_=pos_d[:, tsl].to_broadcast([128, TB])),
                     writes=["posi"], dma_key="d_pos")
                P.op("gpsimd", lambda e: e.tensor_copy(out=rt["f1"][:, :], in_=posi[:, :]), reads=["posi"], writes=["rf1"])
                P.op("gpsimd", lambda e: e.tensor_scalar(out=rt["ts"][:, :], in0=rt["f1"][:, :], scalar1=cst[:, 0:1], scalar2=0.5,
                                                         op0=ALU.mult, op1=ALU.add), reads=["rf1", "cst"], writes=["rts"])
                P.op("gpsimd", lambda e: e.tensor_scalar(out=rt["tc"][:, :], in0=rt["ts"][:, :], scalar1=0.25, scalar2=None,
                                                         op0=ALU.add), reads=["rts"], writes=["rtc"])
                for (src, fr, mk, ii, dst) in (("ts", "f1", "m1", 0, "sin"), ("tc", "f2", "m2", 1, "cos")):
                    P.op("vector", lambda e, src=src, ii=ii: e.tensor_copy(out=rti[ii][:, :], in_=rt[src][:, :]),
                         reads=["r" + src], writes=[f"rti{ii}"])
                    P.op("vector", lambda e, fr=fr, ii=ii: e.tensor_copy(out=rt[fr][:, :], in_=rti[ii][:, :]),
                         reads=[f"rti{ii}"], writes=["r" + fr])
                    P.op("vector", lambda e, src=src, fr=fr: e.tensor_tensor(out=rt[fr][:, :], in0=rt[src][:, :], in1=rt[fr][:, :],
                                                                           op=ALU.subtract), reads=["r" + src, "r" + fr], writes=["r" + fr])
                    P.op("vector", lambda e, fr=fr, mk=mk: e.tensor_scalar(out=rt[mk][:, :], in0=rt[fr][:, :], scalar1=0.0, scalar2=None,
                                                                         op0=ALU.is_lt), reads=["r" + fr], writes=["r" + mk])
                    P.op("vector", lambda e, fr=fr, mk=mk: e.tensor_tensor(out=rt[fr][:, :], in0=rt[fr][:, :], in1=rt[mk][:, :],
                                                                         op=ALU.add), reads=["r" + fr, "r" + mk], writes=["r" + fr])
                    P.op("scalar", lambda e, fr=fr, dst=dst: e.activation(out=rt[dst][:, :], in_=rt[fr][:, :], func=AF.Sin,
                                                                        scale=6.283185, bias=negpi[:, 0:1]),
                         reads=["r" + fr], writes=["r" + dst])

            def rnn_part(m, tsl, sl, c):
                if True:
                    s2 = c % 2
                    bu = proj(m, c * 128, "u")
                    P.op("scalar", lambda e, c=c, bu=bu: e.activation(out=ub[:, c, 3:TB + 3], in_=ps[bu][:, 0:TB], func=AF.Copy),
                         reads=[f"pp{bu}"], writes=[f"ub{c}"])
                    P.op("vector", lambda e, c=c: e.tensor_scalar(
                        out=uc[:, c, :], in0=ub[:, c, 0:TB], scalar1=vcol(V_CONVW, c), scalar2=vcol(V_CONVB, c),
                        op0=ALU.mult, op1=ALU.add), reads=[f"ub{c}"], writes=[f"uc{c}"])
                    for t in range(1, 4):
                        P.op("vector", lambda e, c=c, t=t: e.scalar_tensor_tensor(
                            out=uc[:, c, :], in0=ub[:, c, t:t + TB], scalar=vcol(V_CONVW + 4 * t, c), in1=uc[:, c, :],
                            op0=ALU.mult, op1=ALU.add), reads=[f"ub{c}", f"uc{c}"], writes=[f"uc{c}"])
                    P.op("gpsimd", lambda e, c=c: e.tensor_copy(out=ub[:, c, 0:3], in_=ub[:, c, TB:TB + 3]),
                         reads=[f"ub{c}"], writes=[f"ub{c}"])
                    P.op("scalar", lambda e, c=c: e.activation(out=ucb[:, c, :], in_=uc[:, c, :], func=AF.Copy),
                         reads=[f"uc{c}"], writes=[f"ucb{c}"])
                    P.op("tensor", lambda e, c=c: e.matmul(ps[4 + s2][:, 0:TB], lhsT=wab[:, c, :], rhs=ucb[:, c, :], start=True, stop=True),
                         reads=[f"ucb{c}", "wab"], writes=[f"pra{s2}"])
                    P.op("tensor", lambda e, c=c: e.matmul(ps[6 + s2][:, 0:TB], lhsT=wxb[:, c, :], rhs=ucb[:, c, :], start=True, stop=True),
                         reads=[f"ucb{c}", "wxb"], writes=[f"pri{s2}"])
                    rr, ii_, aa, a2, mu, inp, hs, gx, yr = (tmp[n][s2] for n in ("rr", "ii", "aa", "a2", "mu", "inp", "hs", "gx", "yr"))
                    P.op("scalar", lambda e, c=c, rr=rr: e.activation(out=rr[:, :], in_=ps[4 + s2][:, 0:TB], func=AF.Sigmoid,
                                                                      bias=vcol(V_BA, c), scale=1.0), reads=[f"pra{s2}"], writes=[f"rr{s2}"])
                    P.op("scalar", lambda e, c=c, ii_=ii_: e.activation(out=ii_[:, :], in_=ps[6 + s2][:, 0:TB], func=AF.Sigmoid,
                                                                        bias=vcol(V_BX, c), scale=1.0), reads=[f"pri{s2}"], writes=[f"ii{s2}"])
                    P.op("scalar", lambda e, c=c, rr=rr, aa=aa: e.activation(out=aa[:, :], in_=rr[:, :], func=AF.Exp,
                                                                             scale=clt[:, 4 + c:5 + c]), reads=[f"rr{s2}", "clt1"], writes=[f"aa{s2}"])
                    P.op("scalar", lambda e, c=c, rr=rr, a2=a2: e.activation(out=a2[:, :], in_=rr[:, :], func=AF.Exp,
                                                                             scale=clt[:, 8 + c:9 + c]), reads=[f"rr{s2}", "clt2"], writes=[f"a2{s2}"])
                    P.op("gpsimd", lambda e, a2=a2, mu=mu: e.tensor_scalar(out=mu[:, :], in0=a2[:, :], scalar1=-1.0, scalar2=1.0,
                                                                           op0=ALU.mult, op1=ALU.add), reads=[f"a2{s2}"], writes=[f"mu{s2}"])
                    P.op("scalar", lambda e, mu=mu: e.activation(out=mu[:, :], in_=mu[:, :], func=AF.Ln), reads=[f"mu{s2}"], writes=[f"mu{s2}"])
                    P.op("scalar", lambda e, mu=mu: e.activation(out=mu[:, :], in_=mu[:, :], func=AF.Exp, scale=0.5), reads=[f"mu{s2}"], writes=[f"mu{s2}"])
                    P.op("vector", lambda e, c=c, ii_=ii_, inp=inp: e.tensor_tensor(out=inp[:, :], in0=ii_[:, :], in1=uc[:, c, :], op=ALU.mult),
                         reads=[f"ii{s2}", f"uc{c}"], writes=[f"inp{s2}"])
                    P.op("gpsimd", lambda e, mu=mu, inp=inp: e.tensor_tensor(out=inp[:, :], in0=inp[:, :], in1=mu[:, :], op=ALU.mult),
                         reads=[f"inp{s2}", f"mu{s2}"], writes=[f"inp{s2}"])
                    P.op("vector", lambda e, c=c, aa=aa, inp=inp, hs=hs: e.tensor_tensor_scan(
                        out=hs[:, :], data0=aa[:, :], data1=inp[:, :], initial=carry[:, c:c + 1], op0=ALU.mult, op1=ALU.add),
                        reads=[f"aa{s2}", f"inp{s2}", f"carry{c}"], writes=[f"hs{s2}"])
                    P.op("vector", lambda e, c=c, hs=hs: e.tensor_copy(out=carry[:, c:c + 1], in_=hs[:, TB - 1:TB]),
                         reads=[f"hs{s2}"], writes=[f"carry{c}"])
                    bg = proj(m, 512 + c * 128, "g")
                    P.op("scalar", lambda e, bg=bg, gx=gx: e.activation(out=gx[:, :], in_=ps[bg][:, 0:TB], func=AF.Gelu_apprx_tanh),
                         reads=[f"pp{bg}"], writes=[f"gx{s2}"])
                    P.op("gpsimd", lambda e, hs=hs, gx=gx, yr=yr: e.tensor_tensor(out=yr[:, :], in0=hs[:, :], in1=gx[:, :], op=ALU.mult),
                         reads=[f"hs{s2}", f"gx{s2}"], writes=[f"yr{s2}"])
                    P.op("sync", lambda e, c=c, yr=yr: e.dma_start(out=yT_v[:, c, tsl], in_=yr[:, :]),
                         reads=[f"yr{s2}"], dma_key=f"d_yr{s2}")

            def qk_part(m, tsl, sl, which, c):
                col0, dstb = (1024, qr) if which == "q" else (1536, kr)
                if True:
                    if True:
                        s2 = c % 2
                        t1, t2 = tmp["t1"][s2], tmp["t2"][s2]
                        bq = proj(m, col0 + c * 128, which)
                        hqb = P.op("scalar", lambda e, bq=bq, s2=s2: e.activation(out=qb[s2][:, :], in_=ps[bq][:, 0:TB], func=AF.Copy),
                             reads=[f"pp{bq}"], writes=[f"qb{s2}"])
                        if 'sw' not in SKIP: P.op("tensor", lambda e, s2=s2: e.matmul(ps[6 + s2][:, 0:TB], lhsT=psw[:, :], rhs=qb[s2][:, :], start=True, stop=True),
                             reads=[f"qb{s2}", "psw"], writes=[f"psw{s2}"])
                        if 't1' not in SKIP: P.op("vector", lambda e, bq=bq, t1=t1: e.tensor_tensor(out=t1[:, :], in0=ps[bq][:, 0:TB], in1=rt["cos"][:, :], op=ALU.mult),
                             reads=[f"pp{bq}", "rcos"], writes=[f"t1{s2}"], deps=[hqb])
                        if 't2' not in SKIP: P.op("vector", lambda e, s2=s2, t2=t2: e.tensor_tensor(out=t2[:, :], in0=ps[6 + s2][:, 0:TB], in1=rt["sin"][:, :], op=ALU.mult),
                             reads=[f"psw{s2}", "rsin"], writes=[f"t2{s2}"])
                        if which == "q":
                            P.op("gpsimd", lambda e, c=c, t1=t1, t2=t2: e.tensor_tensor(out=qr[:, c, :], in0=t1[:, :], in1=t2[:, :], op=ALU.add),
                                 reads=[f"t1{s2}", f"t2{s2}"], writes=[f"qr{c}"])
                        else:
                            P.op("gpsimd", lambda e, t1=t1, t2=t2: e.tensor_tensor(out=t1[:, :], in0=t1[:, :], in1=t2[:, :], op=ALU.add),
                                 reads=[f"t1{s2}", f"t2{s2}"], writes=[f"t1{s2}"])
                            P.op("scalar", lambda e, c=c, t1=t1: e.activation(out=kr[:, c, :], in_=t1[:, :], func=AF.Copy),
                                 reads=[f"t1{s2}"], writes=[f"kr{c}"])
                            if 'km' not in SKIP: P.op("vector", lambda e, c=c, t1=t1: e.tensor_reduce(
                                out=kmT[:, c, 2 * m:2 * m + 2], in_=t1[:, :].rearrange("p (b t) -> p b t", b=2), axis=AX.X, op=ALU.add),
                                reads=[f"t1{s2}"], writes=[f"km{c}"])

            def qk_store(m, tsl, which):
                if True:
                    dstb = qr if which == "q" else kr
                    dv = qT_v if which == "q" else kT_v
                    P.op("sync", lambda e, dv=dv, dstb=dstb: e.dma_start(out=dv[:, :, tsl], in_=dstb[:, :, :]),
                         reads=[f"{which}r{c}" for c in range(4)], dma_key=f"d_{which}r")

            def v_part(m, tsl, sl):
                for tc in range(4):
                    b = nbank()

                    def mmv(e, tc=tc, b=b):
                        ins = None
                        for k in range(NK):
                            ins = e.matmul(ps[b][:, 0:512], lhsT=hb[:, k, tc * 128:(tc + 1) * 128], rhs=win[:, k, 2048:2560],
                                           start=(k == 0), stop=(k == NK - 1))
                        return ins
                    P.op("tensor", mmv, reads=[f"h{k}" for k in range(NK)] + [f"win{k}" for k in range(NK)], writes=[f"pp{b}"])
                    P.op("scalar", lambda e, tc=tc, b=b: e.activation(
                        out=vaug[:, :, tc, 0:64], in_=ps[b][:, 0:512].rearrange("p (h e) -> p h e", h=8), func=AF.Copy),
                        reads=[f"pp{b}"], writes=["vaug"])
                P.op("sync", lambda e: e.dma_start(out=Vs_v[:, :, 4 * m:4 * m + 4, :], in_=vaug[:, :, :, :]),
                     reads=["vaug"], dma_key="d_v")

            load(0)
            for m in range(NTB):
                do_tile(m)
            for c in range(4):
                P.op("vector", lambda e, c=c: e.tensor_scalar(out=kmb[:, c, 0:NBLK], in0=kmT[:, c, :], scalar1=1.0 / 256.0, scalar2=None,
                                                              op0=ALU.mult), reads=[f"km{c}"], writes=["kmb"])
            P.op("sync", lambda e: e.dma_start(out=kmD.rearrange("(c p) n -> p c n", p=128), in_=kmb[:, :, :]),
                 reads=["kmb"], dma_key="d_km")
            P.barrier()

    def attn_phase():
        NQT = S // 512
        with ExitStack() as pes:
            sbt = lambda name, shape, dt=F32: pes.enter_context(nc.sbuf_tensor("b2" + name, shape, dt))
            ps = [pes.enter_context(nc.psum_tensor(f"b2ps{i}", [128, 512], F32)) for i in range(7)]
            ptr = pes.enter_context(nc.psum_tensor("b2ptr", [128, 512], BF16))
            KA = [sbt(f"KA{i}", [96, S], BF16) for i in range(2)]
            QA = [sbt(f"QA{i}", [96, S], BF16) for i in range(2)]
            VA = [sbt(f"VA{i}", [128, NCH, 128], BF16) for i in range(2)]
            km = [sbt(f"km{i}", [64, 32], BF16) for i in range(2)]
            G = sbt("G", [128, NCH, 32], F32)
            negm = sbt("negm", [128, NCH, 32], F32)
            pastm = sbt("pastm", [128, NCH, 32], F32)
            thr8 = sbt("thr8", [128, NCH, 8], F32)
            ge = sbt("ge", [128, NCH, 32], F32)
            tp = sbt("tp", [128, NCH, 96], BF16)
            cm = sbt("cm", [128, 4, 512], BF16)
            idn = sbt("idn", [128, 128], BF16)
            PT = [sbt(f"PT{i}", [128, 512], BF16) for i in range(4)]
            rcs = sbt("rcs", [128, 512], F32)
            onesr = sbt("onesr", [128, 64], F32)
            osb = [sbt(f"osb{i}", [64, 512], F32) for i in range(2)]
            ya = [sbt(f"ya{i}", [64, 512], F32) for i in range(2)]

            P.op("sync", lambda e: e.dma_start(out=negm[:, :, :], in_=negm_d.rearrange("p (c n) -> p c n", n=32)), writes=["negm"], dma_key="d_negm")
            P.op("sync", lambda e: e.dma_start(out=pastm[:, :, :], in_=pastm_d.rearrange("p (c n) -> p c n", n=32)), writes=["pastm"], dma_key="d_pastm")
            P.op("sync", lambda e: e.dma_start(out=cm[:, :, :], in_=cm_d.rearrange("p (c n) -> p c n", n=512)), writes=["cm"], dma_key="d_cm")
            P.op("sync", lambda e: e.dma_start(out=idn[:, :], in_=ident_d), writes=["idn"], dma_key="d_idn")
            P.op("vector", lambda e: e.memset(tp[:, :, :], 0.0), writes=["tp"])
            P.op("vector", lambda e: e.memset(onesr[:, :], 1.0), writes=["onesr"])
            for i in range(2):
                P.op("sync", lambda e, i=i: e.dma_start(out=KA[i][64:96, :], in_=onehot_d), writes=[f"KAo{i}"], dma_key=f"d_oh{i}")
            kT_h = kT.rearrange("(h d) s -> h d s", d=64)
            qT_h = qT.rearrange("(h d) s -> h d s", d=64)
            km_h = kmD.rearrange("(h d) n -> h d n", d=64)
            yT_a = YT.rearrange("(g d) s -> g d s", d=64)
            scale = 0.125

            def load_head(h):
                hs_ = h % 2
                P.op("sync", lambda e: e.dma_start(out=KA[hs_][0:64, :], in_=kT_h[h]), writes=[f"KA{hs_}"], dma_key=f"d_ka{hs_}")
                P.op("sync", lambda e: e.dma_start(out=QA[hs_][0:64, :], in_=qT_h[h]), writes=[f"QA{hs_}"], dma_key=f"d_qa{hs_}")
                P.op("sync", lambda e: e.dma_start(out=VA[hs_][:, :, :], in_=Vs[h]), writes=[f"VA{hs_}"], dma_key=f"d_va{hs_}")
                P.op("sync", lambda e: e.dma_start(out=km[hs_][:, :], in_=km_h[h]), writes=[f"km{hs_}"], dma_key=f"d_kmh{hs_}")

            def gating(h):
                hs_ = h % 2
                for grp in range(NCH // 16):
                    def mmg(e, grp=grp):
                        ins = None
                        for cc in range(16):
                            c = grp * 16 + cc
                            ins = e.matmul(ps[6][:, cc * 32:(cc + 1) * 32], lhsT=QA[hs_][0:64, c * 128:(c + 1) * 128],
                                           rhs=km[hs_][0:64, :], start=True, stop=True)
                        return ins
                    P.op("tensor", mmg, reads=[f"QA{hs_}", f"km{hs_}"], writes=["pgt"])
                    P.op("vector", lambda e, grp=grp: e.tensor_tensor(
                        out=G[:, grp * 16:(grp + 1) * 16, :], in0=ps[6][:, 0:512].rearrange("p (c n) -> p c n", n=32),
                        in1=negm[:, grp * 16:(grp + 1) * 16, :], op=ALU.add), reads=["pgt", "negm"], writes=["G"])
                for c in range(NCH):
                    P.op("vector", lambda e, c=c: e.max(out=thr8[:, c, :], in_=G[:, c, :]), reads=["G"], writes=["thr8"])
                P.op("vector", lambda e: e.tensor_tensor(out=ge[:, :, :], in0=G[:, :, :],
                                                         in1=thr8[:, :, 2:3].to_broadcast([128, NCH, 32]), op=ALU.is_ge),
                     reads=["G", "thr8"], writes=["ge"])
                P.op("vector", lambda e: e.scalar_tensor_tensor(out=tp[:, :, 64:96], in0=ge[:, :, :], scalar=-1.0, in1=pastm[:, :, :],
                                                                op0=ALU.add, op1=ALU.mult), reads=["ge", "pastm", "tp"], writes=["tp"])
                for qt in range(NQT):
                    def trs(e, qt=qt):
                        ins = None
                        for cc in range(4):
                            ins = e.transpose(ptr[0:96, cc * 128:(cc + 1) * 128], tp[:, qt * 4 + cc, :], idn[:, :])
                        return ins
                    P.op("tensor", trs, reads=["tp", "idn"], writes=["ptr"])
                    P.op("vector", lambda e, qt=qt: e.tensor_copy(out=QA[hs_][64:96, qt * 512:(qt + 1) * 512], in_=ptr[64:96, :]),
                         reads=["ptr"], writes=[f"QAb{hs_}"])

            def do_head(h):
                hs_ = h % 2
                if h + 1 < 8:
                    load_head(h + 1)
                items = [(m, j) for m in range(NQT) for j in range(4 * m + 4)]
                LA = 3

                def qk_stage(idx):
                    m, j = items[idx]
                    b = idx % 4
                    P.op("tensor", lambda e: e.matmul(
                        ps[b][:, :], lhsT=KA[hs_][0:96, j * 128:(j + 1) * 128], rhs=QA[hs_][0:96, m * 512:(m + 1) * 512],
                        start=True, stop=True), reads=[f"KA{hs_}", f"KAo{hs_}", f"QA{hs_}", f"QAb{hs_}"], writes=[f"pS{b}"])
                    P.op("scalar", lambda e: e.activation(out=PT[b][:, :], in_=ps[b][:, :], func=AF.Exp, scale=scale),
                         reads=[f"pS{b}"], writes=[f"PT{b}"])
                    if j >= 4 * m:
                        P.op("vector", lambda e: e.tensor_tensor(out=PT[b][:, :], in0=PT[b][:, :], in1=cm[:, j - 4 * m, :], op=ALU.mult),
                             reads=[f"PT{b}", "cm"], writes=[f"PT{b}"])

                def pv_stage(idx):
                    m, j = items[idx]
                    b = idx % 4
                    nj = 4 * m + 4
                    po = ps[4 + m % 2]
                    o2 = m % 2
                    P.op("tensor", lambda e: e.matmul(po[:, :], lhsT=VA[hs_][:, j, :], rhs=PT[b][:, :], start=(j == 0), stop=(j == nj - 1)),
                         reads=[f"PT{b}", f"VA{hs_}"], writes=[f"pO{o2}"])
                    if j == nj - 1:
                        hrc = P.op("vector", lambda e: e.reciprocal(out=rcs[64:65, :], in_=po[64:65, :]), reads=[f"pO{o2}"], writes=["rcs"])
                        P.op("tensor", lambda e: e.matmul(ps[6][0:64, :], lhsT=onesr[64:65, 0:64], rhs=rcs[64:65, :], start=True, stop=True),
                             reads=["rcs", "onesr"], writes=["pgt"])
                        P.op("vector", lambda e: e.tensor_copy(out=osb[o2][:, :], in_=po[0:64, :]),
                             reads=[f"pO{o2}"], writes=[f"osb{o2}"], deps=[hrc])
                        P.op("vector", lambda e: e.tensor_tensor(out=ya[o2][:, :], in0=osb[o2][:, :], in1=ps[6][0:64, :], op=ALU.mult),
                             reads=[f"osb{o2}", "pgt"], writes=[f"ya{o2}"])
                        P.op("sync", lambda e: e.dma_start(out=yT_a[8 + h][:, m * 512:(m + 1) * 512], in_=ya[o2][:, :]),
                             reads=[f"ya{o2}"], dma_key=f"d_ya{o2}")
                for idx in range(len(items) + LA):
                    if idx < len(items):
                        qk_stage(idx)
                    if idx >= LA:
                        pv_stage(idx - LA)
                    if idx == len(items) // 2 and h + 1 < 8:
                        gating(h + 1)
            load_head(0)
            gating(0)
            for h in range(8):
                do_head(h)
            P.barrier()

    def outproj_phase(dst):
        eps = LN_EPS / (ALPHA * ALPHA)
        with ExitStack() as pes:
            sbt = lambda name, shape, dt=F32: pes.enter_context(nc.sbuf_tensor("c1" + name, shape, dt))
            ps = [pes.enter_context(nc.psum_tensor(f"c1ps{i}", [128, 512], F32)) for i in range(8)]
            wo = sbt("wo", [128, NK, D], BF16)
            ones_r = sbt("ones_r", [128, 128], BF16)
            yb = [sbt(f"yb{i}", [128, NK, TB], F32) for i in range(2)]
            xb = [sbt(f"xb{i}", [128, NK, TB], F32) for i in range(2)]
            sq = [sbt(f"sq{i}", [128, TB], BF16) for i in range(2)]
            yn = sbt("yn", [128, NK, TB], BF16)
            rs = [sbt(f"rs{i}", [128, TB], F32) for i in range(2)]
            zb = [sbt(f"zb{i}", [128, TB], BF16) for i in range(2)]
            zq = [sbt(f"zq{i}", [128, TB], BF16) for i in range(2)]
            mean = sbt("mean", [128, TB], F32)
            var = sbt("var", [128, TB], F32)
            rstd = sbt("rstd", [128, TB], F32)
            for kk in range(NK):
                P.op("gpsimd", lambda e, kk=kk: e.dma_start(out=wo[:, kk, :], in_=w_out_d[kk * 128:(kk + 1) * 128, :]),
                     writes=[f"wo{kk}"], dma_key=f"d_wo{kk}")
            P.op("vector", lambda e: e.memset(ones_r[:, :], 1.0 / 512.0), writes=["ones_r"])
            y_v = YT.rearrange("(k p) s -> p k s", p=128)
            x_v = x1T.rearrange("(k p) s -> p k s", p=128)
            dst_v = dst.rearrange("(k p) s -> p k s", p=128)
            gn = lambda k: vecs[:, V_GR + k:V_GR + k + 1]
            lg_c = lambda k: vecs[:, V_LN + 16 + k:V_LN + 17 + k]
            lb_c = lambda k: vecs[:, V_LN + 24 + k:V_LN + 25 + k]

            def load(m):
                sl = m % 2
                P.op("sync", lambda e: e.dma_start(out=yb[sl][:, :, :], in_=y_v[:, :, m * TB:(m + 1) * TB]),
                     writes=[f"y{sl}_{k}" for k in range(NK)], dma_key=f"d_y{sl}")
                P.op("sync", lambda e: e.dma_start(out=xb[sl][:, :, :], in_=x_v[:, :, m * TB:(m + 1) * TB]),
                     writes=[f"x{sl}_{k}" for k in range(NK)], dma_key=f"d_x{sl}")

            def front(m):
                sl = m % 2
                for k in range(NK):
                    P.op("scalar", lambda e, k=k: e.activation(out=sq[k % 2][:, :], in_=yb[sl][:, k, :], func=AF.Square),
                         reads=[f"y{sl}_{k}"], writes=[f"sq{k % 2}"])
                    g = k // 4
                    P.op("tensor", lambda e, k=k, g=g: e.matmul(ps[g][:, 0:TB], lhsT=ones_r[:, :], rhs=sq[k % 2][:, :],
                                                               start=(k % 4 == 0), stop=(k % 4 == 3)),
                         reads=[f"sq{k % 2}", "ones_r"], writes=[f"pss{g}"])
                for g in range(2):
                    P.op("vector", lambda e, g=g: e.tensor_scalar(out=rs[g][:, :], in0=ps[g][:, 0:TB], scalar1=RMS_EPS, scalar2=None, op0=ALU.add),
                         reads=[f"pss{g}"], writes=[f"rs{g}"])
                    P.op("scalar", lambda e, g=g: e.activation(out=rs[g][:, :], in_=rs[g][:, :], func=AF.Ln), reads=[f"rs{g}"], writes=[f"rs{g}"])
                    P.op("scalar", lambda e, g=g: e.activation(out=rs[g][:, :], in_=rs[g][:, :], func=AF.Exp, scale=-0.5), reads=[f"rs{g}"], writes=[f"rs{g}"])
                for k in range(NK):
                    P.op("vector", lambda e, k=k: e.scalar_tensor_tensor(out=yn[:, k, :], in0=yb[sl][:, k, :], scalar=gn(k), in1=rs[k // 4][:, :],
                                                                       op0=ALU.mult, op1=ALU.mult),
                         reads=[f"y{sl}_{k}", f"rs{k // 4}"], writes=[f"yn{k}"])

            def do_tile(m):
                sl = m % 2
                if m + 1 < NTB:
                    load(m + 1)

                def stats(dk):
                    P.op("tensor", lambda e, dk=dk: e.matmul(ps[6][:, 0:TB], lhsT=ones_ln[:, :], rhs=zb[dk % 2][:, :],
                                                             start=(dk == 0), stop=(dk == NK - 1)),
                         reads=[f"zb{dk % 2}", "ones_ln"], writes=["pmean"])
                    P.op("tensor", lambda e, dk=dk: e.matmul(ps[7][:, 0:TB], lhsT=ones_ln[:, :], rhs=zq[dk % 2][:, :],
                                                             start=(dk == 0), stop=(dk == NK - 1)),
                         reads=[f"zq{dk % 2}", "ones_ln"], writes=["pez2"])
                for dk in range(NK):
                    py = ps[2 + dk % 2]

                    def mmo(e, dk=dk, py=py):
                        ins = None
                        for k in range(NK):
                            ins = e.matmul(py[:, 0:TB], lhsT=wo[:, k, dk * 128:(dk + 1) * 128], rhs=yn[:, k, :],
                                           start=(k == 0), stop=(k == NK - 1))
                        return ins
                    P.op("tensor", mmo, reads=[f"yn{k}" for k in range(NK)] + [f"wo{k}" for k in range(NK)], writes=[f"py{dk % 2}"])
                    P.op("vector", lambda e, dk=dk, py=py: e.scalar_tensor_tensor(
                        out=xb[sl][:, dk, :], in0=py[:, 0:TB], scalar=gsc[:, 8 + dk:9 + dk], in1=xb[sl][:, dk, :],
                        op0=ALU.mult, op1=ALU.add), reads=[f"py{dk % 2}", f"x{sl}_{dk}"], writes=[f"x{sl}_{dk}"])
                    P.op("scalar", lambda e, dk=dk: e.activation(out=zb[dk % 2][:, :], in_=xb[sl][:, dk, :], func=AF.Copy),
                         reads=[f"x{sl}_{dk}"], writes=[f"zb{dk % 2}"])
                    P.op("scalar", lambda e, dk=dk: e.activation(out=zq[dk % 2][:, :], in_=xb[sl][:, dk, :], func=AF.Square),
                         reads=[f"x{sl}_{dk}"], writes=[f"zq{dk % 2}"])
                    if dk >= 1:
                        stats(dk - 1)
                stats(NK - 1)
                P.capture()
                if m + 1 < NTB:
                    front(m + 1)
                fr_ops = P.end_capture()
                P.capture()
                tail(m, sl)
                tl_ops = P.end_capture()
                P.replay_interleaved([fr_ops, tl_ops] if fr_ops else [tl_ops])

            def tail(m, sl):
                P.op("scalar", lambda e: e.activation(out=mean[:, :], in_=ps[6][:, 0:TB], func=AF.Copy), reads=["pmean"], writes=["mean"])
                P.op("vector", lambda e: e.tensor_tensor(out=var[:, :], in0=mean[:, :], in1=mean[:, :], op=ALU.mult), reads=["mean"], writes=["var"])
                P.op("vector", lambda e: e.scalar_tensor_tensor(out=var[:, :], in0=ps[7][:, 0:TB], scalar=eps, in1=var[:, :],
                                                                op0=ALU.add, op1=ALU.subtract), reads=["pez2", "var"], writes=["var"])
                P.op("scalar", lambda e: e.activation(out=rstd[:, :], in_=var[:, :], func=AF.Ln), reads=["var"], writes=["rstd"])
                P.op("scalar", lambda e: e.activation(out=rstd[:, :], in_=rstd[:, :], func=AF.Exp, scale=-0.5), reads=["rstd"], writes=["rstd"])
                for dk in range(NK):
                    P.op("vector", lambda e, dk=dk: e.tensor_tensor(out=xb[sl][:, dk, :], in0=xb[sl][:, dk, :], in1=mean[:, :], op=ALU.subtract),
                         reads=[f"x{sl}_{dk}", "mean"], writes=[f"x{sl}_{dk}"])
                for dk in range(NK):
                    P.op("vector", lambda e, dk=dk: e.tensor_tensor(out=xb[sl][:, dk, :], in0=xb[sl][:, dk, :], in1=rstd[:, :], op=ALU.mult),
                         reads=[f"x{sl}_{dk}", "rstd"], writes=[f"x{sl}_{dk}"])
                for dk in range(NK):
                    if dk % 2:
                        P.op("gpsimd", lambda e, dk=dk: e.tensor_scalar(out=xb[sl][:, dk, :], in0=xb[sl][:, dk, :], scalar1=lg_c(dk), scalar2=lb_c(dk),
                                                                        op0=ALU.mult, op1=ALU.add), reads=[f"x{sl}_{dk}"], writes=[f"x{sl}_{dk}"])
                    else:
                        P.op("scalar", lambda e, dk=dk: e.activation(out=xb[sl][:, dk, :], in_=xb[sl][:, dk, :], func=AF.Identity,
                                                                     scale=lg_c(dk), bias=lb_c(dk)), reads=[f"x{sl}_{dk}"], writes=[f"x{sl}_{dk}"])
                P.op("sync", lambda e: e.dma_start(out=dst_v[:, :, m * TB:(m + 1) * TB], in_=xb[sl][:, :, :]),
                     reads=[f"x{sl}_{k}" for k in range(NK)], dma_key=f"d_o{sl}")
            load(0)
            front(0)
            for m in range(NTB):
                do_tile(m)
            P.barrier()

    last = stop_after
    ffn_phase("A", xT, outT if last == "A" else x1T, w1g, w1u, w1d, 0, V_LN)
    if last != "A":
        mixer_proj_phase()
        if last != "B1":
            attn_phase()
            if last != "B2":
                outproj_phase(outT if last == "C1" else x2T)
                if last != "C1":
                    ffn_phase("C", x2T, outT, w2g, w2u, w2d, 2, V_LN + 32)

    P.barrier()
    P.emit(es)
    es.close()
    return nc


def pack_cols(v):
    v = np.asarray(v, np.float32).reshape(-1, 128)
    return np.ascontiguousarray(v.T)


def make_consts(S):
    import ml_dtypes
    bf = ml_dtypes.bfloat16
    nch = S // 128
    p = np.arange(128)
    d = p % 64
    invf = np.where(d < 16, 500000.0 ** (-((d % 8).astype(np.float64)) / 8.0), 0.0)
    cst = np.zeros((128, 4), np.float32)
    cst[:, 0] = (invf / (2.0 * np.pi)).astype(np.float32)
    psw = np.zeros((128, 128), np.float32)
    for m in range(128):
        dm = m % 64
        if dm < 8:
            psw[m + 8, m] = -1.0
        elif dm < 16:
            psw[m - 8, m] = 1.0
    onehot = np.zeros((32, S), np.float32)
    for n in range(S // 256):
        onehot[n, n * 256:(n + 1) * 256] = 32768.0
    c = np.arange(nch)[:, None]
    n = np.arange(32)[None, :]
    past = (n < (c // 2)).astype(np.float32)
    negm = np.where(past > 0, 0.0, -1e30).astype(np.float32)
    f = np.arange(512)[None, None, :]
    dj = np.arange(4)[None, :, None]
    cm = (f >= dj * 128 + p[:, None, None]).astype(np.float32)
    return {
        "cst": cst,
        "pswap": psw.astype(bf),
        "ident": np.eye(128, dtype=np.float32).astype(bf),
        "onehot": onehot.astype(bf),
        "negm": np.ascontiguousarray(np.broadcast_to(negm.reshape(1, -1), (128, nch * 32))),
        "pastm": np.ascontiguousarray(np.broadcast_to(past.reshape(1, -1), (128, nch * 32))),
        "cm": np.ascontiguousarray(cm.reshape(128, 4 * 512)).astype(bf),
    }


def make_in_maps(inputs, S=SEQ, ncores=NCORES):
    g = lambda k: np.asarray(inputs[k])
    f32c = lambda a: np.ascontiguousarray(a, dtype=np.float32)
    cols = [pack_cols(g("ada_b")[0])]
    for k in ("ln1_g", "ln1_b", "ln2_g", "ln2_b", "ln3_g", "ln3_b"):
        cols.append(pack_cols(g(k)[0]))
    cw = g("conv_w")[0]
    for t in range(4):
        cols.append(pack_cols(cw[t]))
    for k in ("conv_b", "lru_ba", "lru_bx", "lru_lambda", "norm_rnn_g", "norm_attn_g"):
        cols.append(pack_cols(g(k)[0]))
    vecs = np.ascontiguousarray(np.concatenate(cols, axis=1), dtype=np.float32)
    assert vecs.shape == (128, NV), vecs.shape
    shared = {
        "ada_w": f32c(g("ada_w")[0]),
        "vecs": vecs,
        "w1g": f32c(g("ffn1_w_gate")[0]),
        "w1u": f32c(g("ffn1_w_up")[0]),
        "w1d": f32c(g("ffn1_w_down")[0]),
        "w2g": f32c(g("ffn2_w_gate")[0]),
        "w2u": f32c(g("ffn2_w_up")[0]),
        "w2d": f32c(g("ffn2_w_down")[0]),
        "w_in": f32c(g("w_in")[0]),
        "w_out": f32c(g("w_out")[0]),
        "lru_wa": f32c(g("lru_wa")[0]),
        "lru_wx": f32c(g("lru_wx")[0]),
    }
    shared.update(make_consts(S))
    maps = []
    x = g("x")
    c = g("c")
    pos = g("positions")
    for b in range(ncores):
        m = dict(shared)
        m["xT"] = np.ascontiguousarray(x[b, :S, :].T, dtype=np.float32)
        m["ccol"] = pack_cols(c[b])
        m["pos"] = np.ascontiguousarray(pos[b:b + 1, :S], dtype=np.int32)
        maps.append(m)
    return maps


def kernel(**inputs):
    nc = build_nc(SEQ)
    maps = make_in_maps(inputs)
    res = run_bass_kernel_spmd(nc, maps, core_ids=list(range(NCORES)))
    out = np.stack([np.ascontiguousarray(r["outT"].T) for r in res.results], axis=0)
    return out.astype(np.float32)
```

```python
import numpy as np
from contextlib import ExitStack
import concourse.bass as bass
import concourse.mybir as mybir
from concourse.bass_utils import run_bass_kernel_spmd

F32 = mybir.dt.float32
BF16 = mybir.dt.bfloat16
I32 = mybir.dt.int32
AF = mybir.ActivationFunctionType
ALU = mybir.AluOpType
AX = mybir.AxisListType

D = 1024
DFF = 2816
NF = DFF // 128
NK = D // 128
N_IN = 2560
ALPHA = 2.0 ** 0.25
LN_EPS = 1e-5
RMS_EPS = 1e-6
SEQ = 8192
NCORES = 8
SKIP = set()

V_ADAB = 0
V_LN = 72
V_CONVW = 120
V_CONVB = 136
V_BA = 140
V_BX = 144
V_LAM = 148
V_GR = 152
V_GA = 156
NV = 160


class Hd:
    __slots__ = ("key", "val", "needed", "eng", "is_dma")


class Ph:
    __slots__ = ("real",)


class Prog:
    ENG = ("sync", "scalar", "vector", "gpsimd", "tensor")

    def __init__(self, nc):
        self.nc = nc
        self.streams = {e: [] for e in self.ENG}
        self.state = {}
        self.dmas = []
        self.last = {}
        self.keymap = {}
        self.dcnt = {}

    def capture(self):
        self._cap = []

    def end_capture(self):
        lst, self._cap = self._cap, None
        return lst

    def replay_interleaved(self, lists):
        n = max(len(l) for l in lists)
        for i in range(n):
            for l in lists:
                if i < len(l):
                    ph, eng, fn, reads, writes, dma_key, deps = l[i]
                    rd = [d.real if isinstance(d, Ph) else d for d in deps]
                    ph.real = self.op(eng, fn, reads=reads, writes=writes, dma_key=dma_key, deps=rd)

    def op(self, eng, fn, reads=(), writes=(), dma_key=None, deps=()):
        if getattr(self, "_cap", None) is not None:
            ph = Ph()
            self._cap.append((ph, eng, fn, list(reads), list(writes), dma_key, list(deps)))
            return ph
        h = Hd()
        h.eng = eng
        h.is_dma = dma_key is not None
        h.key = "E_" + eng
        h.needed = False
        h.val = None
        if h.is_dma:
            phys = self.keymap.setdefault(dma_key, len(self.keymap))
            h.key = f"D{phys}"
            self.dcnt[h.key] = self.dcnt.get(h.key, 0) + 16
            h.val = self.dcnt[h.key]
            h.needed = True
        dl = []
        for r in reads:
            st = self.state.get(r)
            if st is not None and st[0] is not None:
                dl.append(st[0])
        for w in writes:
            st = self.state.get(w)
            if st is not None:
                if st[0] is not None:
                    dl.append(st[0])
                dl.extend(st[1].values())
                dl.extend(st[2])
        dl.extend(d for d in deps if d is not None)
        seen = set()
        dd = []
        for d in dl:
            if id(d) not in seen:
                seen.add(id(d))
                d.needed = True
                dd.append(d)
        self.streams[eng].append((fn, dd, h))
        for r in reads:
            st = self.state.setdefault(r, [None, {}, []])
            if h.is_dma:
                st[2].append(h)
            else:
                st[1][eng] = h
        for w in writes:
            self.state[w] = [h, {}, []]
        if h.is_dma:
            self.dmas.append(h)
        elif fn is not None:
            self.last[eng] = h
        return h

    def barrier(self):
        deps = list(self.last.values()) + list(self.dmas)
        self.dmas = []
        for e in self.ENG:
            self.op(e, None, deps=deps)
        self.state = {}
        self.keymap = {}

    def emit(self, es):
        nc = self.nc
        cnt = {}
        for e in self.ENG:
            for (fn, dd, h) in self.streams[e]:
                if h.needed and not h.is_dma:
                    cnt[h.key] = cnt.get(h.key, 0) + 1
                    h.val = cnt[h.key]
        sems = {k: es.enter_context(nc.semaphore("s_" + k)) for k in list(cnt) + list(self.dcnt)}
        streams = self.streams

        def mk(ename):
            def body(eng):
                waited = {}
                for (fn, dd, h) in streams[ename]:
                    need = {}
                    for d in dd:
                        if waited.get(d.key, 0) < d.val:
                            need[d.key] = max(need.get(d.key, 0), d.val)
                    for k, v in need.items():
                        eng.wait_ge(sems[k], v)
                        waited[k] = v
                    if fn is None:
                        continue
                    ins = fn(eng)
                    if h.needed:
                        ins.then_inc(sems[h.key], 16 if h.is_dma else 1)
            return body

        with nc.Block() as blk:
            blk.sync(mk("sync"))
            blk.scalar(mk("scalar"))
            blk.vector(mk("vector"))
            blk.gpsimd(mk("gpsimd"))
            blk.tensor(mk("tensor"))


def build_nc(S=SEQ, debug=False, stop_after=None):
    nc = bass.Bass("TRN2", target_bir_lowering=False)
    P = Prog(nc)
    es = ExitStack()
    okind = "ExternalOutput" if debug else "Internal"

    def din(name, shape, dt=F32):
        return nc.dram_tensor(name, list(shape), dt, kind="ExternalInput").ap()

    xT = din("xT", [D, S])
    ccol = din("ccol", [128, NK])
    ada_w = din("ada_w", [D, 9 * D])
    vecs_d = din("vecs", [128, NV])
    w1g = din("w1g", [D, DFF])
    w1u = din("w1u", [D, DFF])
    w1d = din("w1d", [DFF, D])
    w2g = din("w2g", [D, DFF])
    w2u = din("w2u", [D, DFF])
    w2d = din("w2d", [DFF, D])
    w_in_d = din("w_in", [D, N_IN])
    w_out_d = din("w_out", [D, D])
    lru_wa_d = din("lru_wa", [8, 64, 64])
    lru_wx_d = din("lru_wx", [8, 64, 64])
    pos_d = din("pos", [1, S], I32)
    cst_d = din("cst", [128, 4])
    pswap_d = din("pswap", [128, 128], BF16)
    ident_d = din("ident", [128, 128], BF16)
    onehot_d = din("onehot", [32, S], BF16)
    negm_d = din("negm", [128, (S // 128) * 32])
    pastm_d = din("pastm", [128, (S // 128) * 32])
    cm_d = din("cm", [128, 4 * 512], BF16)
    outT = nc.dram_tensor("outT", [D, S], F32, kind="ExternalOutput").ap()
    x2T = nc.dram_tensor("x2T", [D, S], F32, kind=okind).ap()
    YT = nc.dram_tensor("YT", [D, S], F32, kind=okind).ap()
    qT = nc.dram_tensor("qT", [512, S], BF16, kind=okind).ap()
    kT = nc.dram_tensor("kT", [512, S], BF16, kind=okind).ap()
    Vs = nc.dram_tensor("Vs", [8, 128, S // 128, 128], BF16, kind=okind).ap()
    kmD = nc.dram_tensor("kmD", [512, 32], BF16, kind=okind).ap()
    x1T = nc.dram_tensor("x1T", [D, S], F32, kind=okind).ap()
    modT_d = nc.dram_tensor("modT_d", [128, 72], F32, kind=okind).ap()

    T = 256
    NT = S // T

    vecs = es.enter_context(nc.sbuf_tensor("vecs_sb", [128, NV], F32))
    modT = es.enter_context(nc.sbuf_tensor("modT", [128, 72], F32))
    opsc = es.enter_context(nc.sbuf_tensor("opsc", [128, 24], F32))
    gsc = es.enter_context(nc.sbuf_tensor("gsc", [128, 24], F32))
    ones_ln = es.enter_context(nc.sbuf_tensor("ones_ln", [128, 128], BF16))
    nhalf = es.enter_context(nc.sbuf_tensor("nhalf", [128, 512], F32))
    negpi = es.enter_context(nc.sbuf_tensor("negpi", [128, 1], F32))

    P.op("sync", lambda e: e.dma_start(out=vecs[:, :], in_=vecs_d), writes=["vecs"], dma_key="d_vecs")
    P.op("gpsimd", lambda e: e.memset(ones_ln[:, :], 1.0 / D), writes=["ones_ln"])
    P.op("gpsimd", lambda e: e.memset(nhalf[:, :], -0.5), writes=["nhalf"])
    P.op("gpsimd", lambda e: e.memset(negpi[:, :], -3.1415925), writes=["negpi"])

    with ExitStack() as ps0:
        ps = [ps0.enter_context(nc.psum_tensor("p0ps0", [128, 512], F32))]
        cc = ps0.enter_context(nc.sbuf_tensor("cc", [128, NK], F32))
        cact = ps0.enter_context(nc.sbuf_tensor("cact", [128, NK], F32))
        aw = [ps0.enter_context(nc.sbuf_tensor(f"aw{i}", [128, NK, D], F32)) for i in range(2)]
        P.op("sync", lambda e: e.dma_start(out=cc[:, :], in_=ccol), writes=["cc"], dma_key="d_cc")
        P.op("scalar", lambda e: e.activation(out=cact[:, :], in_=cc[:, :], func=AF.Silu),
             reads=["cc"], writes=["cact"])
        aw_d = ada_w.rearrange("(kk p) n -> p kk n", p=128)
        for i in range(9):
            sl = i % 2
            for kk in range(NK):
                P.op("sync", lambda e, sl=sl, i=i, kk=kk: e.dma_start(
                    out=aw[sl][:, kk, :], in_=aw_d[:, kk, i * D:(i + 1) * D]),
                    writes=[f"aw{sl}_{kk}"], dma_key=f"d_aw{sl}_{kk}")

            def mm(e, sl=sl, i=i):
                ins = None
                for c in range(NK):
                    for kk in range(NK):
                        ins = e.matmul(ps[0][:, i * 8 + c:i * 8 + c + 1],
                                       lhsT=aw[sl][:, kk, c * 128:(c + 1) * 128],
                                       rhs=cact[:, kk:kk + 1], start=(kk == 0), stop=(kk == NK - 1))
                return ins
            P.op("tensor", mm, reads=["cact"] + [f"aw{sl}_{kk}" for kk in range(NK)], writes=["ps0"])
        P.op("vector", lambda e: e.tensor_tensor(out=modT[:, :], in0=ps[0][:, 0:72],
                                                 in1=vecs[:, V_ADAB:V_ADAB + 72], op=ALU.add),
             reads=["ps0", "vecs"], writes=["modT"])
        for i in range(3):
            P.op("vector", lambda e, i=i: e.tensor_scalar(
                out=opsc[:, i * 8:(i + 1) * 8], in0=modT[:, (3 * i + 1) * 8:(3 * i + 2) * 8],
                scalar1=1.0, scalar2=None, op0=ALU.add), reads=["modT"], writes=[f"opsc{i}"])
            gmul = (1.0 if i == 1 else 0.5) / ALPHA
            P.op("vector", lambda e, i=i, gmul=gmul: e.tensor_scalar(
                out=gsc[:, i * 8:(i + 1) * 8], in0=modT[:, (3 * i + 2) * 8:(3 * i + 3) * 8],
                scalar1=1.0, scalar2=gmul, op0=ALU.add, op1=ALU.mult), reads=["modT"], writes=[f"gsc{i}"])
        if debug:
            P.op("sync", lambda e: e.dma_start(out=modT_d, in_=modT[:, :]), reads=["modT"], dma_key="d_dbg")
        P.barrier()

    def ffn_phase(tag, src, dst, wg_d, wu_d, wd_d, mi, ln_col):
        sh_c = lambda k: modT[:, (3 * mi) * 8 + k:(3 * mi) * 8 + k + 1]
        sc_c = lambda k: opsc[:, mi * 8 + k:mi * 8 + k + 1]
        g_c = lambda k: gsc[:, mi * 8 + k:mi * 8 + k + 1]
        lg_c = lambda k: vecs[:, ln_col + k:ln_col + k + 1]
        lb_c = lambda k: vecs[:, ln_col + 8 + k:ln_col + 8 + k + 1]
        eps = LN_EPS / (ALPHA * ALPHA)
        with ExitStack() as pes:
            ps = [pes.enter_context(nc.psum_tensor(tag + f"ps{i}", [128, 512], F32)) for i in range(8)]
            wg = pes.enter_context(nc.sbuf_tensor(tag + "wg", [128, NK, DFF], BF16))
            wu = pes.enter_context(nc.sbuf_tensor(tag + "wu", [128, NK, DFF], BF16))
            wd = pes.enter_context(nc.sbuf_tensor(tag + "wd", [128, NF, D], BF16))
            xb = [pes.enter_context(nc.sbuf_tensor(tag + f"xb{i}", [128, NK, T], F32)) for i in range(2)]
            hb = pes.enter_context(nc.sbuf_tensor(tag + "hb", [128, NK, T], BF16))
            act = pes.enter_context(nc.sbuf_tensor(tag + "act", [128, NF, T], BF16))
            sgt = [pes.enter_context(nc.sbuf_tensor(tag + f"sg{i}", [128, T], BF16)) for i in range(2)]
            zb = [pes.enter_context(nc.sbuf_tensor(tag + f"zb{i}", [128, T], BF16)) for i in range(2)]
            zq = [pes.enter_context(nc.sbuf_tensor(tag + f"zq{i}", [128, T], BF16)) for i in range(2)]
            mean = pes.enter_context(nc.sbuf_tensor(tag + "mean", [128, T], F32))
            var = pes.enter_context(nc.sbuf_tensor(tag + "var", [128, T], F32))
            rstd = pes.enter_context(nc.sbuf_tensor(tag + "rstd", [128, T], F32))

            for kk in range(NK):
                P.op("gpsimd", lambda e, kk=kk: e.dma_start(out=wg[:, kk, :], in_=wg_d[kk * 128:(kk + 1) * 128, :]),
                     writes=[f"wg{kk}"], dma_key=f"d_wg{kk}")
                P.op("gpsimd", lambda e, kk=kk: e.dma_start(out=wu[:, kk, :], in_=wu_d[kk * 128:(kk + 1) * 128, :]),
                     writes=[f"wu{kk}"], dma_key=f"d_wu{kk}")
            for j in range(NF):
                P.op("gpsimd", lambda e, j=j: e.dma_start(out=wd[:, j, :], in_=wd_d[j * 128:(j + 1) * 128, :]),
                     writes=[f"wd{j}"], dma_key=f"d_wd{j}")
            src_v = src.rearrange("(k p) s -> p k s", p=128)
            dst_v = dst.rearrange("(k p) s -> p k s", p=128)

            def load(m):
                sl = m % 2
                P.op("sync", lambda e: e.dma_start(out=xb[sl][:, :, :], in_=src_v[:, :, m * T:(m + 1) * T]),
                     writes=[f"x{sl}_{k}" for k in range(NK)], dma_key=f"d_x{sl}")

            def make_h(m):
                sl = m % 2
                for k in range(NK):
                    P.op("gpsimd", lambda e, k=k: e.tensor_scalar(
                        out=hb[:, k, :], in0=xb[sl][:, k, :], scalar1=sc_c(k), scalar2=sh_c(k),
                        op0=ALU.mult, op1=ALU.add),
                        reads=[f"x{sl}_{k}", "modT", "opsc0", "opsc2"], writes=[f"h{k}"])

            load(0)
            make_h(0)
            def do_tile(m):
                sl = m % 2
                if m + 1 < NT:
                    load(m + 1)
                for j in range(NF):
                    pg = ps[j % 2]
                    pu = ps[2 + j % 2]

                    def mmg(e, j=j, pg=pg):
                        ins = None
                        for k in range(NK):
                            ins = e.matmul(pg[:, 0:T], lhsT=wg[:, k, j * 128:(j + 1) * 128], rhs=hb[:, k, :],
                                           start=(k == 0), stop=(k == NK - 1))
                        return ins

                    def mmu(e, j=j, pu=pu):
                        ins = None
                        for k in range(NK):
                            ins = e.matmul(pu[:, 0:T], lhsT=wu[:, k, j * 128:(j + 1) * 128], rhs=hb[:, k, :],
                                           start=(k == 0), stop=(k == NK - 1))
                        return ins
                    hr = [f"h{k}" for k in range(NK)]
                    P.op("tensor", mmg, reads=hr + [f"wg{k}" for k in range(NK)], writes=[f"pg{j % 2}"])
                    P.op("tensor", mmu, reads=hr + [f"wu{k}" for k in range(NK)], writes=[f"pu{j % 2}"])
                    P.op("scalar", lambda e, j=j, pg=pg: e.activation(out=sgt[j % 2][:, :], in_=pg[:, 0:T], func=AF.Silu),
                         reads=[f"pg{j % 2}"], writes=[f"sg{j % 2}"])
                    P.op("vector", lambda e, j=j, pu=pu: e.tensor_tensor(
                        out=act[:, j, :], in0=sgt[j % 2][:, :], in1=pu[:, 0:T], op=ALU.mult),
                        reads=[f"sg{j % 2}", f"pu{j % 2}"], writes=[f"act{j}"])
                if m + 1 < NT:
                    make_h(m + 1)
                def stats(dk):
                    P.op("tensor", lambda e, dk=dk: e.matmul(ps[6][:, 0:T], lhsT=ones_ln[:, :], rhs=zb[dk % 2][:, :],
                                                             start=(dk == 0), stop=(dk == NK - 1)),
                         reads=[f"zb{dk % 2}", "ones_ln"], writes=["pmean"])
                    P.op("tensor", lambda e, dk=dk: e.matmul(ps[7][:, 0:T], lhsT=ones_ln[:, :], rhs=zq[dk % 2][:, :],
                                                             start=(dk == 0), stop=(dk == NK - 1)),
                         reads=[f"zq{dk % 2}", "ones_ln"], writes=["pez2"])
                for dk in range(NK):
                    py = ps[4 + dk % 2]

                    def mmd(e, dk=dk, py=py):
                        ins = None
                        for j in range(NF):
                            ins = e.matmul(py[:, 0:T], lhsT=wd[:, j, dk * 128:(dk + 1) * 128], rhs=act[:, j, :],
                                           start=(j == 0), stop=(j == NF - 1))
                        return ins
                    P.op("tensor", mmd, reads=[f"act{j}" for j in range(NF)] + [f"wd{j}" for j in range(NF)],
                         writes=[f"py{dk % 2}"])
                    P.op("vector", lambda e, dk=dk, py=py: e.scalar_tensor_tensor(
                        out=xb[sl][:, dk, :], in0=py[:, 0:T], scalar=g_c(dk), in1=xb[sl][:, dk, :],
                        op0=ALU.mult, op1=ALU.add),
                        reads=[f"py{dk % 2}", f"x{sl}_{dk}", "gsc0", "gsc2"], writes=[f"x{sl}_{dk}"])
                    P.op("scalar", lambda e, dk=dk: e.activation(out=zb[dk % 2][:, :], in_=xb[sl][:, dk, :], func=AF.Copy),
                         reads=[f"x{sl}_{dk}"], writes=[f"zb{dk % 2}"])
                    P.op("gpsimd", lambda e, dk=dk: e.tensor_tensor(
                        out=zq[dk % 2][:, :], in0=xb[sl][:, dk, :], in1=xb[sl][:, dk, :], op=ALU.mult),
                        reads=[f"x{sl}_{dk}"], writes=[f"zq{dk % 2}"])
                    if dk >= 1:
                        stats(dk - 1)
                stats(NK - 1)
                P.op("scalar", lambda e: e.activation(out=mean[:, :], in_=ps[6][:, 0:T], func=AF.Copy),
                     reads=["pmean"], writes=["mean"])
                P.op("vector", lambda e: e.tensor_tensor(out=var[:, :], in0=mean[:, :], in1=mean[:, :], op=ALU.mult),
                     reads=["mean"], writes=["var"])
                P.op("vector", lambda e: e.scalar_tensor_tensor(
                    out=var[:, :], in0=ps[7][:, 0:T], scalar=eps, in1=var[:, :], op0=ALU.add, op1=ALU.subtract),
                    reads=["pez2", "var"], writes=["var"])
                P.op("scalar", lambda e: e.activation(out=rstd[:, :], in_=var[:, :], func=AF.Ln), reads=["var"], writes=["rstd"])
                P.op("scalar", lambda e: e.activation(out=rstd[:, :], in_=rstd[:, :], func=AF.Exp, scale=-0.5), reads=["rstd"], writes=["rstd"])
                for dk in range(NK):
                    P.op("vector", lambda e, dk=dk: e.tensor_tensor(
                        out=xb[sl][:, dk, :], in0=xb[sl][:, dk, :], in1=mean[:, :], op=ALU.subtract),
                        reads=[f"x{sl}_{dk}", "mean"], writes=[f"x{sl}_{dk}"])
                for dk in range(NK):
                    P.op("vector", lambda e, dk=dk: e.tensor_tensor(
                        out=xb[sl][:, dk, :], in0=xb[sl][:, dk, :], in1=rstd[:, :], op=ALU.mult),
                        reads=[f"x{sl}_{dk}", "rstd"], writes=[f"x{sl}_{dk}"])
                for dk in range(NK):
                    P.op("gpsimd", lambda e, dk=dk: e.tensor_scalar(
                        out=xb[sl][:, dk, :], in0=xb[sl][:, dk, :], scalar1=lg_c(dk), scalar2=lb_c(dk),
                        op0=ALU.mult, op1=ALU.add),
                        reads=[f"x{sl}_{dk}", "vecs"], writes=[f"x{sl}_{dk}"])
                P.op("sync", lambda e, m=m: e.dma_start(out=dst_v[:, :, m * T:(m + 1) * T], in_=xb[sl][:, :, :]),
                     reads=[f"x{sl}_{k}" for k in range(NK)], dma_key=f"d_o{sl}")
            for m in range(NT):
                do_tile(m)
            P.barrier()

    TB = 512
    NTB = S // TB
    NCH = S // 128
    NBLK = S // 256

    def mixer_proj_phase():
        with ExitStack() as pes:
            sbt = lambda name, shape, dt=F32: pes.enter_context(nc.sbuf_tensor("b1" + name, shape, dt))
            ps = [pes.enter_context(nc.psum_tensor(f"b1ps{i}", [128, 512], F32)) for i in range(8)]
            win = sbt("win", [128, NK, N_IN], BF16)
            wab = sbt("wab", [128, 4, 128], BF16)
            wxb = sbt("wxb", [128, 4, 128], BF16)
            psw = sbt("psw", [128, 128], BF16)
            cst = sbt("cst", [128, 4], F32)
            clt = sbt("clt", [128, 12], F32)
            half = sbt("half", [128, TB], F32)
            xb = [sbt(f"xb{i}", [128, NK, TB], F32) for i in range(2)]
            hb = sbt("hb", [128, NK, TB], BF16)
            posi = sbt("posi", [128, TB], I32)
            rt = {n: sbt("rt" + n, [128, TB], F32) for n in ("ts", "tc", "f1", "f2", "m1", "m2", "sin", "cos")}
            rti = [sbt(f"rti{i}", [128, TB], I32) for i in range(2)]
            ub = sbt("ub", [128, 4, TB + 3], F32)
            uc = sbt("uc", [128, 4, TB], F32)
            ucb = sbt("ucb", [128, 4, TB], BF16)
            tmp = {n: [sbt(f"{n}{i}", [128, TB], F32) for i in range(2)]
                   for n in ("rr", "ii", "aa", "a2", "mu", "inp", "hs", "gx", "yr", "t1", "t2")}
            qb = [sbt(f"qb{i}", [128, TB], BF16) for i in range(2)]
            qr = sbt("qr", [128, 4, TB], BF16)
            kr = sbt("kr", [128, 4, TB], BF16)
            vaug = sbt("vaug", [128, 8, 4, 128], BF16)
            carry = sbt("carry", [128, 4], F32)
            kmT = sbt("kmT", [128, 4, NBLK], F32)
            kmb = sbt("kmb", [128, 4, 32], BF16)

            for kk in range(NK):
                P.op("gpsimd", lambda e, kk=kk: e.dma_start(out=win[:, kk, :], in_=w_in_d[kk * 128:(kk + 1) * 128, :]),
                     writes=[f"win{kk}"], dma_key=f"d_win{kk}")
            P.op("vector", lambda e: e.memset(wab[:, :, :], 0.0), writes=["wab"])
            P.op("vector", lambda e: e.memset(wxb[:, :, :], 0.0), writes=["wxb"])
            for n in range(8):
                c, hh = n // 2, n % 2
                P.op("gpsimd", lambda e, n=n, c=c, hh=hh: e.dma_start(
                    out=wab[hh * 64:(hh + 1) * 64, c, hh * 64:(hh + 1) * 64], in_=lru_wa_d[n, :, :]),
                    reads=[], writes=["wab"], dma_key=f"d_wa{n}")
                P.op("gpsimd", lambda e, n=n, c=c, hh=hh: e.dma_start(
                    out=wxb[hh * 64:(hh + 1) * 64, c, hh * 64:(hh + 1) * 64], in_=lru_wx_d[n, :, :]),
                    reads=[], writes=["wxb"], dma_key=f"d_wx{n}")
            P.op("sync", lambda e: e.dma_start(out=psw[:, :], in_=pswap_d), writes=["psw"], dma_key="d_psw")
            P.op("sync", lambda e: e.dma_start(out=cst[:, :], in_=cst_d), writes=["cst"], dma_key="d_cst")
            P.op("vector", lambda e: e.memset(half[:, :], 0.5), writes=["half"])
            P.op("vector", lambda e: e.memset(ub[:, :, :], 0.0), writes=[f"ub{c}" for c in range(4)])
            P.op("vector", lambda e: e.memset(carry[:, :], 0.0), writes=[f"carry{c}" for c in range(4)])
            P.op("vector", lambda e: e.memset(vaug[:, :, :, :], 1.0), writes=["vaug"])
            P.op("vector", lambda e: e.memset(kmb[:, :, :], 0.0), writes=["kmb"])
            P.op("scalar", lambda e: e.activation(out=clt[:, 0:4], in_=vecs[:, V_LAM:V_LAM + 4], func=AF.Exp, scale=-1.0),
                 reads=["vecs"], writes=["clt0"])
            P.op("scalar", lambda e: e.activation(out=clt[:, 0:4], in_=clt[:, 0:4], func=AF.Ln, bias=1.0, scale=1.0),
                 reads=["clt0"], writes=["clt0"])
            P.op("vector", lambda e: e.tensor_scalar(out=clt[:, 4:8], in0=clt[:, 0:4], scalar1=-8.0, scalar2=None, op0=ALU.mult),
                 reads=["clt0"], writes=["clt1"])
            P.op("vector", lambda e: e.tensor_scalar(out=clt[:, 8:12], in0=clt[:, 0:4], scalar1=-16.0, scalar2=None, op0=ALU.mult),
                 reads=["clt0"], writes=["clt2"])

            src_v = x1T.rearrange("(k p) s -> p k s", p=128)
            yT_v = YT.rearrange("(k p) s -> p k s", p=128)
            qT_v = qT.rearrange("(k p) s -> p k s", p=128)
            kT_v = kT.rearrange("(k p) s -> p k s", p=128)
            Vs_v = Vs.rearrange("h p j e -> p h j e")
            vcol = lambda col, c: vecs[:, col + c:col + c + 1]
            bankc = [0]

            def nbank():
                b = bankc[0] % 4
                bankc[0] += 1
                return b

            def load(m):
                sl = m % 2
                P.op("sync", lambda e: e.dma_start(out=xb[sl][:, :, :], in_=src_v[:, :, m * TB:(m + 1) * TB]),
                     writes=[f"x{sl}_{k}" for k in range(NK)], dma_key=f"d_x{sl}")

            def proj(m, col0, key):
                b = nbank()

                def mm(e, b=b):
                    ins = None
                    for k in range(NK):
                        ins = e.matmul(ps[b][:, 0:TB], lhsT=win[:, k, col0:col0 + 128], rhs=hb[:, k, :],
                                       start=(k == 0), stop=(k == NK - 1))
                    return ins
                P.op("tensor", mm, reads=[f"h{k}" for k in range(NK)] + [f"win{k}" for k in range(NK)], writes=[f"pp{b}"])
                return b

            def do_tile(m):
                sl = m % 2
                if m + 1 < NTB:
                    load(m + 1)
                tsl = slice(m * TB, (m + 1) * TB)
                for k in range(NK):
                    P.op("gpsimd", lambda e, k=k: e.tensor_scalar(
                        out=hb[:, k, :], in0=xb[sl][:, k, :], scalar1=opsc[:, 8 + k:9 + k], scalar2=modT[:, 24 + k:25 + k],
                        op0=ALU.mult, op1=ALU.add), reads=[f"x{sl}_{k}"], writes=[f"h{k}"])
                def cap(f, *a):
                    P.capture()
                    f(*a)
                    return P.end_capture()
                P.replay_interleaved([cap(rope_tables, m, tsl), cap(rnn_part, m, tsl, sl, 0), cap(rnn_part, m, tsl, sl, 1)])
                P.replay_interleaved([cap(rnn_part, m, tsl, sl, 2), cap(rnn_part, m, tsl, sl, 3)])
                for which in ("q", "k"):
                    P.replay_interleaved([cap(qk_part, m, tsl, sl, which, 0), cap(qk_part, m, tsl, sl, which, 1)])
                    P.replay_interleaved([cap(qk_part, m, tsl, sl, which, 2), cap(qk_part, m, tsl, sl, which, 3)])
                    qk_store(m, tsl, which)
                v_part(m, tsl, sl)

            def rope_tables(m, tsl):
                P.op("sync", lambda e: e.dma_start(out=posi[:, :], in_=pos_d[:, tsl].to_broadcast([128, TB])),
                     writes=["posi"], dma_key="d_pos")
                P.op("gpsimd", lambda e: e.tensor_copy(out=rt["f1"][:, :], in_=posi[:, :]), reads=["posi"], writes=["rf1"])
                P.op("gpsimd", lambda e: e.tensor_scalar(out=rt["ts"][:, :], in0=rt["f1"][:, :], scalar1=cst[:, 0:1], scalar2=0.5,
                                                         op0=ALU.mult, op1=ALU.add), reads=["rf1", "cst"], writes=["rts"])
                P.op("gpsimd", lambda e: e.tensor_scalar(out=rt["tc"][:, :], in0=rt["ts"][:, :], scalar1=0.25, scalar2=None,
                                                         op0=ALU.add), reads=["rts"], writes=["rtc"])
                for (src, fr, mk, ii, dst) in (("ts", "f1", "m1", 0, "sin"), ("tc", "f2", "m2", 1, "cos")):
                    P.op("vector", lambda e, src=src, ii=ii: e.tensor_copy(out=rti[ii][:, :], in_=rt[src][:, :]),
                         reads=["r" + src], writes=[f"rti{ii}"])
                    P.op("vector", lambda e, fr=fr, ii=ii: e.tensor_copy(out=rt[fr][:, :], in_=rti[ii][:, :]),
                         reads=[f"rti{ii}"], writes=["r" + fr])
                    P.op("vector", lambda e, src=src, fr=fr: e.tensor_tensor(out=rt[fr][:, :], in0=rt[src][:, :], in1=rt[fr][:, :],
                                                                           op=ALU.subtract), reads=["r" + src, "r" + fr], writes=["r" + fr])
                    P.op("vector", lambda e, fr=fr, mk=mk: e.tensor_scalar(out=rt[mk][:, :], in0=rt[fr][:, :], scalar1=0.0, scalar2=None,
                                                                         op0=ALU.is_lt), reads=["r" + fr], writes=["r" + mk])
                    P.op("vector", lambda e, fr=fr, mk=mk: e.tensor_tensor(out=rt[fr][:, :], in0=rt[fr][:, :], in1=rt[mk][:, :],
                                                                         op=ALU.add), reads=["r" + fr, "r" + mk], writes=["r" + fr])
                    P.op("scalar", lambda e, fr=fr, dst=dst: e.activation(out=rt[dst][:, :], in_=rt[fr][:, :], func=AF.Sin,
                                                                        scale=6.283185, bias=negpi[:, 0:1]),
                         reads=["r" + fr], writes=["r" + dst])

            def rnn_part(m, tsl, sl, c):
                if True:
                    s2 = c % 2
                    bu = proj(m, c * 128, "u")
                    P.op("scalar", lambda e, c=c, bu=bu: e.activation(out=ub[:, c, 3:TB + 3], in_=ps[bu][:, 0:TB], func=AF.Copy),
                         reads=[f"pp{bu}"], writes=[f"ub{c}"])
                    P.op("vector", lambda e, c=c: e.tensor_scalar(
                        out=uc[:, c, :], in0=ub[:, c, 0:TB], scalar1=vcol(V_CONVW, c), scalar2=vcol(V_CONVB, c),
                        op0=ALU.mult, op1=ALU.add), reads=[f"ub{c}"], writes=[f"uc{c}"])
                    for t in range(1, 4):
                        P.op("vector", lambda e, c=c, t=t: e.scalar_tensor_tensor(
                            out=uc[:, c, :], in0=ub[:, c, t:t + TB], scalar=vcol(V_CONVW + 4 * t, c), in1=uc[:, c, :],
                            op0=ALU.mult, op1=ALU.add), reads=[f"ub{c}", f"uc{c}"], writes=[f"uc{c}"])
                    P.op("gpsimd", lambda e, c=c: e.tensor_copy(out=ub[:, c, 0:3], in_=ub[:, c, TB:TB + 3]),
                         reads=[f"ub{c}"], writes=[f"ub{c}"])
                    P.op("scalar", lambda e, c=c: e.activation(out=ucb[:, c, :], in_=uc[:, c, :], func=AF.Copy),
                         reads=[f"uc{c}"], writes=[f"ucb{c}"])
                    P.op("tensor", lambda e, c=c: e.matmul(ps[4 + s2][:, 0:TB], lhsT=wab[:, c, :], rhs=ucb[:, c, :], start=True, stop=True),
                         reads=[f"ucb{c}", "wab"], writes=[f"pra{s2}"])
                    P.op("tensor", lambda e, c=c: e.matmul(ps[6 + s2][:, 0:TB], lhsT=wxb[:, c, :], rhs=ucb[:, c, :], start=True, stop=True),
                         reads=[f"ucb{c}", "wxb"], writes=[f"pri{s2}"])
                    rr, ii_, aa, a2, mu, inp, hs, gx, yr = (tmp[n][s2] for n in ("rr", "ii", "aa", "a2", "mu", "inp", "hs", "gx", "yr"))
                    P.op("scalar", lambda e, c=c, rr=rr: e.activation(out=rr[:, :], in_=ps[4 + s2][:, 0:TB], func=AF.Sigmoid,
                                                                      bias=vcol(V_BA, c), scale=1.0), reads=[f"pra{s2}"], writes=[f"rr{s2}"])
                    P.op("scalar", lambda e, c=c, ii_=ii_: e.activation(out=ii_[:, :], in_=ps[6 + s2][:, 0:TB], func=AF.Sigmoid,
                                                                        bias=vcol(V_BX, c), scale=1.0), reads=[f"pri{s2}"], writes=[f"ii{s2}"])
                    P.op("scalar", lambda e, c=c, rr=rr, aa=aa: e.activation(out=aa[:, :], in_=rr[:, :], func=AF.Exp,
                                                                             scale=clt[:, 4 + c:5 + c]), reads=[f"rr{s2}", "clt1"], writes=[f"aa{s2}"])
                    P.op("scalar", lambda e, c=c, rr=rr, a2=a2: e.activation(out=a2[:, :], in_=rr[:, :], func=AF.Exp,
                                                                             scale=clt[:, 8 + c:9 + c]), reads=[f"rr{s2}", "clt2"], writes=[f"a2{s2}"])
                    P.op("gpsimd", lambda e, a2=a2, mu=mu: e.tensor_scalar(out=mu[:, :], in0=a2[:, :], scalar1=-1.0, scalar2=1.0,
                                                                           op0=ALU.mult, op1=ALU.add), reads=[f"a2{s2}"], writes=[f"mu{s2}"])
                    P.op("scalar", lambda e, mu=mu: e.activation(out=mu[:, :], in_=mu[:, :], func=AF.Ln), reads=[f"mu{s2}"], writes=[f"mu{s2}"])
                    P.op("scalar", lambda e, mu=mu: e.activation(out=mu[:, :], in_=mu[:, :], func=AF.Exp, scale=0.5), reads=[f"mu{s2}"], writes=[f"mu{s2}"])
                    P.op("vector", lambda e, c=c, ii_=ii_, inp=inp: e.tensor_tensor(out=inp[:, :], in0=ii_[:, :], in1=uc[:, c, :], op=ALU.mult),
                         reads=[f"ii{s2}", f"uc{c}"], writes=[f"inp{s2}"])
                    P.op("gpsimd", lambda e, mu=mu, inp=inp: e.tensor_tensor(out=inp[:, :], in0=inp[:, :], in1=mu[:, :], op=ALU.mult),
                         reads=[f"inp{s2}", f"mu{s2}"], writes=[f"inp{s2}"])
                    P.op("vector", lambda e, c=c, aa=aa, inp=inp, hs=hs: e.tensor_tensor_scan(
                        out=hs[:, :], data0=aa[:, :], data1=inp[:, :], initial=carry[:, c:c + 1], op0=ALU.mult, op1=ALU.add),
                        reads=[f"aa{s2}", f"inp{s2}", f"carry{c}"], writes=[f"hs{s2}"])
                    P.op("vector", lambda e, c=c, hs=hs: e.tensor_copy(out=carry[:, c:c + 1], in_=hs[:, TB - 1:TB]),
                         reads=[f"hs{s2}"], writes=[f"carry{c}"])
                    bg = proj(m, 512 + c * 128, "g")
                    P.op("scalar", lambda e, bg=bg, gx=gx: e.activation(out=gx[:, :], in_=ps[bg][:, 0:TB], func=AF.Gelu_apprx_tanh),
                         reads=[f"pp{bg}"], writes=[f"gx{s2}"])
                    P.op("gpsimd", lambda e, hs=hs, gx=gx, yr=yr: e.tensor_tensor(out=yr[:, :], in0=hs[:, :], in1=gx[:, :], op=ALU.mult),
                         reads=[f"hs{s2}", f"gx{s2}"], writes=[f"yr{s2}"])
                    P.op("sync", lambda e, c=c, yr=yr: e.dma_start(out=yT_v[:, c, tsl], in_=yr[:, :]),
                         reads=[f"yr{s2}"], dma_key=f"d_yr{s2}")

            def qk_part(m, tsl, sl, which, c):
                col0, dstb = (1024, qr) if which == "q" else (1536, kr)
                if True:
                    if True:
                        s2 = c % 2
                        t1, t2 = tmp["t1"][s2], tmp["t2"][s2]
                        bq = proj(m, col0 + c * 128, which)
                        hqb = P.op("scalar", lambda e, bq=bq, s2=s2: e.activation(out=qb[s2][:, :], in_=ps[bq][:, 0:TB], func=AF.Copy),
                             reads=[f"pp{bq}"], writes=[f"qb{s2}"])
                        if 'sw' not in SKIP: P.op("tensor", lambda e, s2=s2: e.matmul(ps[6 + s2][:, 0:TB], lhsT=psw[:, :], rhs=qb[s2][:, :], start=True, stop=True),
                             reads=[f"qb{s2}", "psw"], writes=[f"psw{s2}"])
                        if 't1' not in SKIP: P.op("vector", lambda e, bq=bq, t1=t1: e.tensor_tensor(out=t1[:, :], in0=ps[bq][:, 0:TB], in1=rt["cos"][:, :], op=ALU.mult),
                             reads=[f"pp{bq}", "rcos"], writes=[f"t1{s2}"], deps=[hqb])
                        if 't2' not in SKIP: P.op("vector", lambda e, s2=s2, t2=t2: e.tensor_tensor(out=t2[:, :], in0=ps[6 + s2][:, 0:TB], in1=rt["sin"][:, :], op=ALU.mult),
                             reads=[f"psw{s2}", "rsin"], writes=[f"t2{s2}"])
                        if which == "q":
                            P.op("gpsimd", lambda e, c=c, t1=t1, t2=t2: e.tensor_tensor(out=qr[:, c, :], in0=t1[:, :], in1=t2[:, :], op=ALU.add),
                                 reads=[f"t1{s2}", f"t2{s2}"], writes=[f"qr{c}"])
                        else:
                            P.op("gpsimd", lambda e, t1=t1, t2=t2: e.tensor_tensor(out=t1[:, :], in0=t1[:, :], in1=t2[:, :], op=ALU.add),
                                 reads=[f"t1{s2}", f"t2{s2}"], writes=[f"t1{s2}"])
                            P.op("scalar", lambda e, c=c, t1=t1: e.activation(out=kr[:, c, :], in_=t1[:, :], func=AF.Copy),
                                 reads=[f"t1{s2}"], writes=[f"kr{c}"])
                            if 'km' not in SKIP: P.op("vector", lambda e, c=c, t1=t1: e.tensor_reduce(
                                out=kmT[:, c, 2 * m:2 * m + 2], in_=t1[:, :].rearrange("p (b t) -> p b t", b=2), axis=AX.X, op=ALU.add),
                                reads=[f"t1{s2}"], writes=[f"km{c}"])

            def qk_store(m, tsl, which):
                if True:
                    dstb = qr if which == "q" else kr
                    dv = qT_v if which == "q" else kT_v
                    P.op("sync", lambda e, dv=dv, dstb=dstb: e.dma_start(out=dv[:, :, tsl], in_=dstb[:, :, :]),
                         reads=[f"{which}r{c}" for c in range(4)], dma_key=f"d_{which}r")

            def v_part(m, tsl, sl):
                for tc in range(4):
                    b = nbank()

                    def mmv(e, tc=tc, b=b):
                        ins = None
                        for k in range(NK):
                            ins = e.matmul(ps[b][:, 0:512], lhsT=hb[:, k, tc * 128:(tc + 1) * 128], rhs=win[:, k, 2048:2560],
                                           start=(k == 0), stop=(k == NK - 1))
                        return ins
                    P.op("tensor", mmv, reads=[f"h{k}" for k in range(NK)] + [f"win{k}" for k in range(NK)], writes=[f"pp{b}"])
                    P.op("scalar", lambda e, tc=tc, b=b: e.activation(
                        out=vaug[:, :, tc, 0:64], in_=ps[b][:, 0:512].rearrange("p (h e) -> p h e", h=8), func=AF.Copy),
                        reads=[f"pp{b}"], writes=["vaug"])
                P.op("sync", lambda e: e.dma_start(out=Vs_v[:, :, 4 * m:4 * m + 4, :], in_=vaug[:, :, :, :]),
                     reads=["vaug"], dma_key="d_v")

            load(0)
            for m in range(NTB):
                do_tile(m)
            for c in range(4):
                P.op("vector", lambda e, c=c: e.tensor_scalar(out=kmb[:, c, 0:NBLK], in0=kmT[:, c, :], scalar1=1.0 / 256.0, scalar2=None,
                                                              op0=ALU.mult), reads=[f"km{c}"], writes=["kmb"])
            P.op("sync", lambda e: e.dma_start(out=kmD.rearrange("(c p) n -> p c n", p=128), in_=kmb[:, :, :]),
                 reads=["kmb"], dma_key="d_km")
            P.barrier()

    def attn_phase():
        NQT = S // 512
        with ExitStack() as pes:
            sbt = lambda name, shape, dt=F32: pes.enter_context(nc.sbuf_tensor("b2" + name, shape, dt))
            ps = [pes.enter_context(nc.psum_tensor(f"b2ps{i}", [128, 512], F32)) for i in range(7)]
            ptr = pes.enter_context(nc.psum_tensor("b2ptr", [128, 512], BF16))
            KA = [sbt(f"KA{i}", [96, S], BF16) for i in range(2)]
            QA = [sbt(f"QA{i}", [96, S], BF16) for i in range(2)]
            VA = [sbt(f"VA{i}", [128, NCH, 128], BF16) for i in range(2)]
            km = [sbt(f"km{i}", [64, 32], BF16) for i in range(2)]
            G = sbt("G", [128, NCH, 32], F32)
            negm = sbt("negm", [128, NCH, 32], F32)
            pastm = sbt("pastm", [128, NCH, 32], F32)
            thr8 = sbt("thr8", [128, NCH, 8], F32)
            ge = sbt("ge", [128, NCH, 32], F32)
            tp = sbt("tp", [128, NCH, 96], BF16)
            cm = sbt("cm", [128, 4, 512], BF16)
            idn = sbt("idn", [128, 128], BF16)
            PT = [sbt(f"PT{i}", [128, 512], BF16) for i in range(4)]
            rcs = sbt("rcs", [128, 512], F32)
            onesr = sbt("onesr", [128, 64], F32)
            osb = [sbt(f"osb{i}", [64, 512], F32) for i in range(2)]
            ya = [sbt(f"ya{i}", [64, 512], F32) for i in range(2)]

            P.op("sync", lambda e: e.dma_start(out=negm[:, :, :], in_=negm_d.rearrange("p (c n) -> p c n", n=32)), writes=["negm"], dma_key="d_negm")
            P.op("sync", lambda e: e.dma_start(out=pastm[:, :, :], in_=pastm_d.rearrange("p (c n) -> p c n", n=32)), writes=["pastm"], dma_key="d_pastm")
            P.op("sync", lambda e: e.dma_start(out=cm[:, :, :], in_=cm_d.rearrange("p (c n) -> p c n", n=512)), writes=["cm"], dma_key="d_cm")
            P.op("sync", lambda e: e.dma_start(out=idn[:, :], in_=ident_d), writes=["idn"], dma_key="d_idn")
            P.op("vector", lambda e: e.memset(tp[:, :, :], 0.0), writes=["tp"])
            P.op("vector", lambda e: e.memset(onesr[:, :], 1.0), writes=["onesr"])
            for i in range(2):
                P.op("sync", lambda e, i=i: e.dma_start(out=KA[i][64:96, :], in_=onehot_d), writes=[f"KAo{i}"], dma_key=f"d_oh{i}")
            kT_h = kT.rearrange("(h d) s -> h d s", d=64)
            qT_h = qT.rearrange("(h d) s -> h d s", d=64)
            km_h = kmD.rearrange("(h d) n -> h d n", d=64)
            yT_a = YT.rearrange("(g d) s -> g d s", d=64)
            scale = 0.125

            def load_head(h):
                hs_ = h % 2
                P.op("sync", lambda e: e.dma_start(out=KA[hs_][0:64, :], in_=kT_h[h]), writes=[f"KA{hs_}"], dma_key=f"d_ka{hs_}")
                P.op("sync", lambda e: e.dma_start(out=QA[hs_][0:64, :], in_=qT_h[h]), writes=[f"QA{hs_}"], dma_key=f"d_qa{hs_}")
                P.op("sync", lambda e: e.dma_start(out=VA[hs_][:, :, :], in_=Vs[h]), writes=[f"VA{hs_}"], dma_key=f"d_va{hs_}")
                P.op("sync", lambda e: e.dma_start(out=km[hs_][:, :], in_=km_h[h]), writes=[f"km{hs_}"], dma_key=f"d_kmh{hs_}")

            def gating(h):
                hs_ = h % 2
                for grp in range(NCH // 16):
                    def mmg(e, grp=grp):
                        ins = None
                        for cc in range(16):
                            c = grp * 16 + cc
                            ins = e.matmul(ps[6][:, cc * 32:(cc + 1) * 32], lhsT=QA[hs_][0:64, c * 128:(c + 1) * 128],
                                           rhs=km[hs_][0:64, :], start=True, stop=True)
                        return ins
                    P.op("tensor", mmg, reads=[f"QA{hs_}", f"km{hs_}"], writes=["pgt"])
                    P.op("vector", lambda e, grp=grp: e.tensor_tensor(
                        out=G[:, grp * 16:(grp + 1) * 16, :], in0=ps[6][:, 0:512].rearrange("p (c n) -> p c n", n=32),
                        in1=negm[:, grp * 16:(grp + 1) * 16, :], op=ALU.add), reads=["pgt", "negm"], writes=["G"])
                for c in range(NCH):
                    P.op("vector", lambda e, c=c: e.max(out=thr8[:, c, :], in_=G[:, c, :]), reads=["G"], writes=["thr8"])
                P.op("vector", lambda e: e.tensor_tensor(out=ge[:, :, :], in0=G[:, :, :],
                                                         in1=thr8[:, :, 2:3].to_broadcast([128, NCH, 32]), op=ALU.is_ge),
                     reads=["G", "thr8"], writes=["ge"])
                P.op("vector", lambda e: e.scalar_tensor_tensor(out=tp[:, :, 64:96], in0=ge[:, :, :], scalar=-1.0, in1=pastm[:, :, :],
                                                                op0=ALU.add, op1=ALU.mult), reads=["ge", "pastm", "tp"], writes=["tp"])
                for qt in range(NQT):
                    def trs(e, qt=qt):
                        ins = None
                        for cc in range(4):
                            ins = e.transpose(ptr[0:96, cc * 128:(cc + 1) * 128], tp[:, qt * 4 + cc, :], idn[:, :])
                        return ins
                    P.op("tensor", trs, reads=["tp", "idn"], writes=["ptr"])
                    P.op("vector", lambda e, qt=qt: e.tensor_copy(out=QA[hs_][64:96, qt * 512:(qt + 1) * 512], in_=ptr[64:96, :]),
                         reads=["ptr"], writes=[f"QAb{hs_}"])

            def do_head(h):
                hs_ = h % 2
                if h + 1 < 8:
                    load_head(h + 1)
                items = [(m, j) for m in range(NQT) for j in range(4 * m + 4)]
                LA = 3

                def qk_stage(idx):
                    m, j = items[idx]
                    b = idx % 4
                    c0 = max(0, j - 4 * m) * 128
                    P.op("tensor", lambda e: e.matmul(
                        ps[b][:, c0:512], lhsT=KA[hs_][0:96, j * 128:(j + 1) * 128], rhs=QA[hs_][0:96, m * 512 + c0:(m + 1) * 512],
                        start=True, stop=True), reads=[f"KA{hs_}", f"KAo{hs_}", f"QA{hs_}", f"QAb{hs_}"], writes=[f"pS{b}"])
                    P.op("scalar", lambda e: e.activation(out=PT[b][:, c0:512], in_=ps[b][:, c0:512], func=AF.Exp, scale=scale),
                         reads=[f"pS{b}"], writes=[f"PT{b}"])
                    if j >= 4 * m:
                        P.op("vector", lambda e: e.tensor_tensor(out=PT[b][:, c0:512], in0=PT[b][:, c0:512], in1=cm[:, j - 4 * m, c0:512], op=ALU.mult),
                             reads=[f"PT{b}", "cm"], writes=[f"PT{b}"])

                def pv_stage(idx):
                    m, j = items[idx]
                    b = idx % 4
                    nj = 4 * m + 4
                    po = ps[4 + m % 2]
                    o2 = m % 2
                    c0 = max(0, j - 4 * m) * 128
                    P.op("tensor", lambda e: e.matmul(po[:, c0:512], lhsT=VA[hs_][:, j, :], rhs=PT[b][:, c0:512], start=(j == 0), stop=(j == nj - 1)),
                         reads=[f"PT{b}", f"VA{hs_}"], writes=[f"pO{o2}"])
                    if j == nj - 1:
                        hrc = P.op("vector", lambda e: e.reciprocal(out=rcs[64:65, :], in_=po[64:65, :]), reads=[f"pO{o2}"], writes=["rcs"])
                        P.op("tensor", lambda e: e.matmul(ps[6][0:64, :], lhsT=onesr[64:65, 0:64], rhs=rcs[64:65, :], start=True, stop=True),
                             reads=["rcs", "onesr"], writes=["pgt"])
                        P.op("vector", lambda e: e.tensor_copy(out=osb[o2][:, :], in_=po[0:64, :]),
                             reads=[f"pO{o2}"], writes=[f"osb{o2}"], deps=[hrc])
                        P.op("vector", lambda e: e.tensor_tensor(out=ya[o2][:, :], in0=osb[o2][:, :], in1=ps[6][0:64, :], op=ALU.mult),
                             reads=[f"osb{o2}", "pgt"], writes=[f"ya{o2}"])
                        P.op("sync", lambda e: e.dma_start(out=yT_a[8 + h][:, m * 512:(m + 1) * 512], in_=ya[o2][:, :]),
                             reads=[f"ya{o2}"], dma_key=f"d_ya{o2}")
                for idx in range(len(items) + LA):
                    if idx < len(items):
                        qk_stage(idx)
                    if idx >= LA:
                        pv_stage(idx - LA)
                    if idx == len(items) // 2 and h + 1 < 8:
                        gating(h + 1)
            load_head(0)
            gating(0)
            for h in range(8):
                do_head(h)
            P.barrier()

    def outproj_phase(dst):
        eps = LN_EPS / (ALPHA * ALPHA)
        with ExitStack() as pes:
            sbt = lambda name, shape, dt=F32: pes.enter_context(nc.sbuf_tensor("c1" + name, shape, dt))
            ps = [pes.enter_context(nc.psum_tensor(f"c1ps{i}", [128, 512], F32)) for i in range(8)]
            wo = sbt("wo", [128, NK, D], BF16)
            ones_r = sbt("ones_r", [128, 128], BF16)
            yb = [sbt(f"yb{i}", [128, NK, TB], F32) for i in range(2)]
            xb = [sbt(f"xb{i}", [128, NK, TB], F32) for i in range(2)]
            sq = [sbt(f"sq{i}", [128, TB], BF16) for i in range(2)]
            yn = sbt("yn", [128, NK, TB], BF16)
            rs = [sbt(f"rs{i}", [128, TB], F32) for i in range(2)]
            zb = [sbt(f"zb{i}", [128, TB], BF16) for i in range(2)]
            zq = [sbt(f"zq{i}", [128, TB], BF16) for i in range(2)]
            mean = sbt("mean", [128, TB], F32)
            var = sbt("var", [128, TB], F32)
            rstd = sbt("rstd", [128, TB], F32)
            for kk in range(NK):
                P.op("gpsimd", lambda e, kk=kk: e.dma_start(out=wo[:, kk, :], in_=w_out_d[kk * 128:(kk + 1) * 128, :]),
                     writes=[f"wo{kk}"], dma_key=f"d_wo{kk}")
            P.op("vector", lambda e: e.memset(ones_r[:, :], 1.0 / 512.0), writes=["ones_r"])
            y_v = YT.rearrange("(k p) s -> p k s", p=128)
            x_v = x1T.rearrange("(k p) s -> p k s", p=128)
            dst_v = dst.rearrange("(k p) s -> p k s", p=128)
            gn = lambda k: vecs[:, V_GR + k:V_GR + k + 1]
            lg_c = lambda k: vecs[:, V_LN + 16 + k:V_LN + 17 + k]
            lb_c = lambda k: vecs[:, V_LN + 24 + k:V_LN + 25 + k]

            def load(m):
                sl = m % 2
                P.op("sync", lambda e: e.dma_start(out=yb[sl][:, :, :], in_=y_v[:, :, m * TB:(m + 1) * TB]),
                     writes=[f"y{sl}_{k}" for k in range(NK)], dma_key=f"d_y{sl}")
                P.op("sync", lambda e: e.dma_start(out=xb[sl][:, :, :], in_=x_v[:, :, m * TB:(m + 1) * TB]),
                     writes=[f"x{sl}_{k}" for k in range(NK)], dma_key=f"d_x{sl}")

            def front(m):
                sl = m % 2
                for k in range(NK):
                    P.op("scalar", lambda e, k=k: e.activation(out=sq[k % 2][:, :], in_=yb[sl][:, k, :], func=AF.Square),
                         reads=[f"y{sl}_{k}"], writes=[f"sq{k % 2}"])
                    g = k // 4
                    P.op("tensor", lambda e, k=k, g=g: e.matmul(ps[g][:, 0:TB], lhsT=ones_r[:, :], rhs=sq[k % 2][:, :],
                                                               start=(k % 4 == 0), stop=(k % 4 == 3)),
                         reads=[f"sq{k % 2}", "ones_r"], writes=[f"pss{g}"])
                for g in range(2):
                    P.op("vector", lambda e, g=g: e.tensor_scalar(out=rs[g][:, :], in0=ps[g][:, 0:TB], scalar1=RMS_EPS, scalar2=None, op0=ALU.add),
                         reads=[f"pss{g}"], writes=[f"rs{g}"])
                    P.op("scalar", lambda e, g=g: e.activation(out=rs[g][:, :], in_=rs[g][:, :], func=AF.Ln), reads=[f"rs{g}"], writes=[f"rs{g}"])
                    P.op("scalar", lambda e, g=g: e.activation(out=rs[g][:, :], in_=rs[g][:, :], func=AF.Exp, scale=-0.5), reads=[f"rs{g}"], writes=[f"rs{g}"])
                for k in range(NK):
                    P.op("vector", lambda e, k=k: e.scalar_tensor_tensor(out=yn[:, k, :], in0=yb[sl][:, k, :], scalar=gn(k), in1=rs[k // 4][:, :],
                                                                       op0=ALU.mult, op1=ALU.mult),
                         reads=[f"y{sl}_{k}", f"rs{k // 4}"], writes=[f"yn{k}"])

            def do_tile(m):
                sl = m % 2
                if m + 1 < NTB:
                    load(m + 1)

                def stats(dk):
                    P.op("tensor", lambda e, dk=dk: e.matmul(ps[6][:, 0:TB], lhsT=ones_ln[:, :], rhs=zb[dk % 2][:, :],
                                                             start=(dk == 0), stop=(dk == NK - 1)),
                         reads=[f"zb{dk % 2}", "ones_ln"], writes=["pmean"])
                    P.op("tensor", lambda e, dk=dk: e.matmul(ps[7][:, 0:TB], lhsT=ones_ln[:, :], rhs=zq[dk % 2][:, :],
                                                             start=(dk == 0), stop=(dk == NK - 1)),
                         reads=[f"zq{dk % 2}", "ones_ln"], writes=["pez2"])
                for dk in range(NK):
                    py = ps[2 + dk % 2]

                    def mmo(e, dk=dk, py=py):
                        ins = None
                        for k in range(NK):
                            ins = e.matmul(py[:, 0:TB], lhsT=wo[:, k, dk * 128:(dk + 1) * 128], rhs=yn[:, k, :],
                                           start=(k == 0), stop=(k == NK - 1))
                        return ins
                    P.op("tensor", mmo, reads=[f"yn{k}" for k in range(NK)] + [f"wo{k}" for k in range(NK)], writes=[f"py{dk % 2}"])
                    P.op("vector", lambda e, dk=dk, py=py: e.scalar_tensor_tensor(
                        out=xb[sl][:, dk, :], in0=py[:, 0:TB], scalar=gsc[:, 8 + dk:9 + dk], in1=xb[sl][:, dk, :],
                        op0=ALU.mult, op1=ALU.add), reads=[f"py{dk % 2}", f"x{sl}_{dk}"], writes=[f"x{sl}_{dk}"])
                    P.op("scalar", lambda e, dk=dk: e.activation(out=zb[dk % 2][:, :], in_=xb[sl][:, dk, :], func=AF.Copy),
                         reads=[f"x{sl}_{dk}"], writes=[f"zb{dk % 2}"])
                    P.op("scalar", lambda e, dk=dk: e.activation(out=zq[dk % 2][:, :], in_=xb[sl][:, dk, :], func=AF.Square),
                         reads=[f"x{sl}_{dk}"], writes=[f"zq{dk % 2}"])
                    if dk >= 1:
                        stats(dk - 1)
                stats(NK - 1)
                P.capture()
                if m + 1 < NTB:
                    front(m + 1)
                fr_ops = P.end_capture()
                P.capture()
                tail(m, sl)
                tl_ops = P.end_capture()
                P.replay_interleaved([fr_ops, tl_ops] if fr_ops else [tl_ops])

            def tail(m, sl):
                P.op("scalar", lambda e: e.activation(out=mean[:, :], in_=ps[6][:, 0:TB], func=AF.Copy), reads=["pmean"], writes=["mean"])
                P.op("vector", lambda e: e.tensor_tensor(out=var[:, :], in0=mean[:, :], in1=mean[:, :], op=ALU.mult), reads=["mean"], writes=["var"])
                P.op("vector", lambda e: e.scalar_tensor_tensor(out=var[:, :], in0=ps[7][:, 0:TB], scalar=eps, in1=var[:, :],
                                                                op0=ALU.add, op1=ALU.subtract), reads=["pez2", "var"], writes=["var"])
                P.op("scalar", lambda e: e.activation(out=rstd[:, :], in_=var[:, :], func=AF.Ln), reads=["var"], writes=["rstd"])
                P.op("scalar", lambda e: e.activation(out=rstd[:, :], in_=rstd[:, :], func=AF.Exp, scale=-0.5), reads=["rstd"], writes=["rstd"])
                for dk in range(NK):
                    P.op("vector", lambda e, dk=dk: e.tensor_tensor(out=xb[sl][:, dk, :], in0=xb[sl][:, dk, :], in1=mean[:, :], op=ALU.subtract),
                         reads=[f"x{sl}_{dk}", "mean"], writes=[f"x{sl}_{dk}"])
                for dk in range(NK):
                    P.op("vector", lambda e, dk=dk: e.tensor_tensor(out=xb[sl][:, dk, :], in0=xb[sl][:, dk, :], in1=rstd[:, :], op=ALU.mult),
                         reads=[f"x{sl}_{dk}", "rstd"], writes=[f"x{sl}_{dk}"])
                for dk in range(NK):
                    if dk % 2:
                        P.op("gpsimd", lambda e, dk=dk: e.tensor_scalar(out=xb[sl][:, dk, :], in0=xb[sl][:, dk, :], scalar1=lg_c(dk), scalar2=lb_c(dk),
                                                                        op0=ALU.mult, op1=ALU.add), reads=[f"x{sl}_{dk}"], writes=[f"x{sl}_{dk}"])
                    else:
                        P.op("scalar", lambda e, dk=dk: e.activation(out=xb[sl][:, dk, :], in_=xb[sl][:, dk, :], func=AF.Identity,
                                                                     scale=lg_c(dk), bias=lb_c(dk)), reads=[f"x{sl}_{dk}"], writes=[f"x{sl}_{dk}"])
                P.op("sync", lambda e: e.dma_start(out=dst_v[:, :, m * TB:(m + 1) * TB], in_=xb[sl][:, :, :]),
                     reads=[f"x{sl}_{k}" for k in range(NK)], dma_key=f"d_o{sl}")
            load(0)
            front(0)
            for m in range(NTB):
                do_tile(m)
            P.barrier()

    last = stop_after
    ffn_phase("A", xT, outT if last == "A" else x1T, w1g, w1u, w1d, 0, V_LN)
    if last != "A":
        mixer_proj_phase()
        if last != "B1":
            attn_phase()
            if last != "B2":
                outproj_phase(outT if last == "C1" else x2T)
                if last != "C1":
                    ffn_phase("C", x2T, outT, w2g, w2u, w2d, 2, V_LN + 32)

    P.barrier()
    P.emit(es)
    es.close()
    return nc


def pack_cols(v):
    v = np.asarray(v, np.float32).reshape(-1, 128)
    return np.ascontiguousarray(v.T)


def make_consts(S):
    import ml_dtypes
    bf = ml_dtypes.bfloat16
    nch = S // 128
    p = np.arange(128)
    d = p % 64
    invf = np.where(d < 16, 500000.0 ** (-((d % 8).astype(np.float64)) / 8.0), 0.0)
    cst = np.zeros((128, 4), np.float32)
    cst[:, 0] = (invf / (2.0 * np.pi)).astype(np.float32)
    psw = np.zeros((128, 128), np.float32)
    for m in range(128):
        dm = m % 64
        if dm < 8:
            psw[m + 8, m] = -1.0
        elif dm < 16:
            psw[m - 8, m] = 1.0
    onehot = np.zeros((32, S), np.float32)
    for n in range(S // 256):
        onehot[n, n * 256:(n + 1) * 256] = 32768.0
    c = np.arange(nch)[:, None]
    n = np.arange(32)[None, :]
    past = (n < (c // 2)).astype(np.float32)
    negm = np.where(past > 0, 0.0, -1e30).astype(np.float32)
    f = np.arange(512)[None, None, :]
    dj = np.arange(4)[None, :, None]
    cm = (f >= dj * 128 + p[:, None, None]).astype(np.float32)
    return {
        "cst": cst,
        "pswap": psw.astype(bf),
        "ident": np.eye(128, dtype=np.float32).astype(bf),
        "onehot": onehot.astype(bf),
        "negm": np.ascontiguousarray(np.broadcast_to(negm.reshape(1, -1), (128, nch * 32))),
        "pastm": np.ascontiguousarray(np.broadcast_to(past.reshape(1, -1), (128, nch * 32))),
        "cm": np.ascontiguousarray(cm.reshape(128, 4 * 512)).astype(bf),
    }


def make_in_maps(inputs, S=SEQ, ncores=NCORES):
    g = lambda k: np.asarray(inputs[k])
    f32c = lambda a: np.ascontiguousarray(a, dtype=np.float32)
    cols = [pack_cols(g("ada_b")[0])]
    for k in ("ln1_g", "ln1_b", "ln2_g", "ln2_b", "ln3_g", "ln3_b"):
        cols.append(pack_cols(g(k)[0]))
    cw = g("conv_w")[0]
    for t in range(4):
        cols.append(pack_cols(cw[t]))
    for k in ("conv_b", "lru_ba", "lru_bx", "lru_lambda", "norm_rnn_g", "norm_attn_g"):
        cols.append(pack_cols(g(k)[0]))
    vecs = np.ascontiguousarray(np.concatenate(cols, axis=1), dtype=np.float32)
    assert vecs.shape == (128, NV), vecs.shape
    shared = {
        "ada_w": f32c(g("ada_w")[0]),
        "vecs": vecs,
        "w1g": f32c(g("ffn1_w_gate")[0]),
        "w1u": f32c(g("ffn1_w_up")[0]),
        "w1d": f32c(g("ffn1_w_down")[0]),
        "w2g": f32c(g("ffn2_w_gate")[0]),
        "w2u": f32c(g("ffn2_w_up")[0]),
        "w2d": f32c(g("ffn2_w_down")[0]),
        "w_in": f32c(g("w_in")[0]),
        "w_out": f32c(g("w_out")[0]),
        "lru_wa": f32c(g("lru_wa")[0]),
        "lru_wx": f32c(g("lru_wx")[0]),
    }
    shared.update(make_consts(S))
    maps = []
    x = g("x")
    c = g("c")
    pos = g("positions")
    for b in range(ncores):
        m = dict(shared)
        m["xT"] = np.ascontiguousarray(x[b, :S, :].T, dtype=np.float32)
        m["ccol"] = pack_cols(c[b])
        m["pos"] = np.ascontiguousarray(pos[b:b + 1, :S], dtype=np.int32)
        maps.append(m)
    return maps


def kernel(**inputs):
    nc = build_nc(SEQ)
    maps = make_in_maps(inputs)
    res = run_bass_kernel_spmd(nc, maps, core_ids=list(range(NCORES)))
    out = np.stack([np.ascontiguousarray(r["outT"].T) for r in res.results], axis=0)
    return out.astype(np.float32)
```
